# Optimizing a Trainium2 kernel written in Bass

```python
import math
import jax, jax.numpy as jnp
from jax import lax
import numpy as np

D_MODEL = 1024
BATCH = 2
SEQ = 8192
DEPTH = 2

HEAD_DIM = 64
MIX_W = D_MODEL
MIX_HALF = MIX_W // 2
FOX_HEADS = MIX_HALF // HEAD_DIM
FOX_W = FOX_HEADS * HEAD_DIM
SSD_HD = HEAD_DIM
SSD_HEADS = MIX_HALF // SSD_HD
SSD_W = SSD_HEADS * SSD_HD
SSD_GROUPS = 2
SSD_STATE = 64
SSD_CONV = 4
SSD_CONV_DIM = SSD_W + 2 * SSD_GROUPS * SSD_STATE
SSD_CHUNK = 128
SB_HEADS = MIX_HALF // HEAD_DIM
SB_W = SB_HEADS * HEAD_DIM
GLA_HEADS = 4
GLA_DV = MIX_HALF // GLA_HEADS
GLA_DK = GLA_DV // 2
GLA_RANK = 16
GLA_CHUNK = 16
GLA_GATE_NORM = 16.0
D_FF = 4 * D_MODEL
Q_BLOCK = 128
EPS = 1e-5
N_EVEN = (DEPTH + 1) // 2
N_ODD = DEPTH // 2

AB_SPLITS = (FOX_W, FOX_W, FOX_W, FOX_HEADS, SSD_W, SSD_CONV_DIM, SSD_HEADS)
AB_IN = sum(AB_SPLITS)
CD_SPLITS = (SB_W, SB_W, SB_W, GLA_HEADS * GLA_DK, GLA_HEADS * GLA_DK,
             GLA_HEADS * GLA_DV, GLA_RANK, GLA_HEADS * GLA_DV)
CD_IN = sum(CD_SPLITS)

kernel_name = 'hybrid_fox_ssd_stickbreak_gla_trunk'


def _split(h, sizes):
    idx = [int(i) for i in np.cumsum(sizes)[:-1]]
    return jnp.split(h, idx, axis=-1)


def rms_norm(x, w):
    xf = x.astype(jnp.float32)
    y = xf * lax.rsqrt(jnp.mean(xf * xf, axis=-1, keepdims=True) + EPS)
    return (y * w.astype(jnp.float32)).astype(x.dtype)


def _to_heads(t, n_heads):
    b, l, _ = t.shape
    return t.reshape(b, l, n_heads, -1).transpose(0, 2, 1, 3)


def _from_heads(t):
    b, h, l, d = t.shape
    return t.transpose(0, 2, 1, 3).reshape(b, l, h * d)


def _gather_blocks(out):
    nb, b, h, q, d = out.shape
    return out.transpose(1, 2, 0, 3, 4).reshape(b, h, nb * q, d)


def forgetting_attention(q, k, v, log_f):
    L = q.shape[2]
    scale = HEAD_DIM ** -0.5
    F = jnp.cumsum(log_f, axis=-1)
    key_pos = jnp.arange(L)

    def block(i):
        start = i * Q_BLOCK
        qb = lax.dynamic_slice_in_dim(q, start, Q_BLOCK, axis=2)
        Fb = lax.dynamic_slice_in_dim(F, start, Q_BLOCK, axis=2)
        s = jnp.einsum('bhqd,bhkd->bhqk', qb, k).astype(jnp.float32) * scale
        s = s + Fb[..., :, None] - F[..., None, :]
        qpos = start + jnp.arange(Q_BLOCK)
        mask = qpos[:, None] >= key_pos[None, :]
        p = jax.nn.softmax(jnp.where(mask, s, -jnp.inf), axis=-1)
        return jnp.einsum('bhqk,bhkd->bhqd', p.astype(v.dtype), v)

    return _gather_blocks(lax.map(block, jnp.arange(L // Q_BLOCK)))


def stick_breaking_attention(q, k, v):
    L = q.shape[2]
    scale = HEAD_DIM ** -0.5
    key_pos = jnp.arange(L)

    def block(i):
        start = i * Q_BLOCK
        qb = lax.dynamic_slice_in_dim(q, start, Q_BLOCK, axis=2)
        z = jnp.einsum('bhqd,bhkd->bhqk', qb, k).astype(jnp.float32) * scale
        qpos = start + jnp.arange(Q_BLOCK)
        mask = key_pos[None, :] < qpos[:, None]
        log_beta = jax.nn.log_sigmoid(z)
        log_rest = jnp.where(mask, jax.nn.log_sigmoid(-z), 0.0)
        suffix = lax.cumsum(log_rest, axis=3, reverse=True) - log_rest
        a = jnp.where(mask, jnp.exp(log_beta + suffix), 0.0)
        return jnp.einsum('bhqk,bhkd->bhqd', a.astype(v.dtype), v)

    return _gather_blocks(lax.map(block, jnp.arange(L // Q_BLOCK)))


def causal_depthwise_conv(x, w, b):
    c = x.shape[-1]
    y = lax.conv_general_dilated(
        x, w[:, None, :].astype(x.dtype), window_strides=(1,),
        padding=[(w.shape[0] - 1, 0)], dimension_numbers=('NWC', 'WIO', 'NWC'),
        feature_group_count=c)
    return y + b.astype(x.dtype)


def _chunk_recurrence(decay, local):
    def step(s, inp):
        d, loc = inp
        return s * d + loc, s
    init = jnp.zeros_like(local[:, 0])
    _, prev = lax.scan(step, init, (jnp.moveaxis(decay, 1, 0), jnp.moveaxis(local, 1, 0)))
    return jnp.moveaxis(prev, 0, 1)


def ssd_chunked(x, dt, A, Bm, Cm, d_skip):
    b, L, H, P = x.shape
    rep = H // SSD_GROUPS
    Q = SSD_CHUNK
    nc = L // Q
    Bh = jnp.repeat(Bm, rep, axis=2).reshape(b, nc, Q, H, SSD_STATE)
    Ch = jnp.repeat(Cm, rep, axis=2).reshape(b, nc, Q, H, SSD_STATE)
    xc = x.reshape(b, nc, Q, H, P)
    dtc = dt.reshape(b, nc, Q, H)
    xdt = xc * dtc[..., None]
    a_cum = jnp.cumsum(dtc * A, axis=2)
    causal = (jnp.arange(Q)[:, None] >= jnp.arange(Q)[None, :])[:, :, None]
    seg = a_cum[:, :, :, None, :] - a_cum[:, :, None, :, :]
    lmat = jnp.exp(jnp.where(causal, seg, -jnp.inf))
    scores = jnp.einsum('bclhn,bcshn->bclsh', Ch, Bh) * lmat
    y_diag = jnp.einsum('bclsh,bcshp->bclhp', scores, xdt)
    decay_to_end = jnp.exp(a_cum[:, :, -1:, :] - a_cum)
    local = jnp.einsum('bcshn,bcsh,bcshp->bchpn', Bh, decay_to_end, xdt)
    chunk_decay = jnp.exp(a_cum[:, :, -1, :])[..., None, None]
    prev = _chunk_recurrence(chunk_decay, local)
    y_off = jnp.einsum('bclhn,bchpn,bclh->bclhp', Ch, prev, jnp.exp(a_cum))
    y = y_diag + y_off + d_skip[None, None, None, :, None] * xc
    return y.reshape(b, L, H, P)


def gla_chunked(q, k, v, log_a):
    b, L, H, K = q.shape
    V = v.shape[-1]
    Q = GLA_CHUNK
    nc = L // Q
    qc = q.reshape(b, nc, Q, H, K)
    kc = k.reshape(b, nc, Q, H, K)
    vc = v.reshape(b, nc, Q, H, V)
    g = jnp.cumsum(log_a.reshape(b, nc, Q, H, K), axis=2)
    causal = (jnp.arange(Q)[:, None] >= jnp.arange(Q)[None, :])[:, :, None, None]
    rel = g[:, :, :, None] - g[:, :, None, :]
    decay = jnp.exp(jnp.where(causal, rel, -jnp.inf))
    attn = jnp.einsum('bcthk,bcshk,bctshk->bctsh', qc, kc, decay)
    o_intra = jnp.einsum('bctsh,bcshv->bcthv', attn, vc)
    g_last = g[:, :, -1]
    k_dec = kc * jnp.exp(g_last[:, :, None] - g)
    local = jnp.einsum('bcshk,bcshv->bchkv', k_dec, vc)
    prev = _chunk_recurrence(jnp.exp(g_last)[..., None], local)
    o_inter = jnp.einsum('bcthk,bchkv->bcthv', qc * jnp.exp(g), prev)
    return (o_intra + o_inter).reshape(b, L, H, V)


def mixer_fox_ssd(h, w_in, f_bias, conv_w, conv_b, dt_bias, a_log, d_skip, ssd_norm_w, w_out):
    b, L, _ = h.shape
    proj = h @ w_in
    fq, fk, fv, f_logit, z, xbc, dt_raw = _split(proj, AB_SPLITS)
    log_f = jax.nn.log_sigmoid((f_logit + f_bias).astype(jnp.float32)).transpose(0, 2, 1)
    y_fox = forgetting_attention(_to_heads(fq, FOX_HEADS), _to_heads(fk, FOX_HEADS),
                                 _to_heads(fv, FOX_HEADS), log_f)
    y_fox = _from_heads(y_fox)
    xbc = jax.nn.silu(causal_depthwise_conv(xbc, conv_w, conv_b))
    xs, bm, cm = _split(xbc, (SSD_W, SSD_GROUPS * SSD_STATE, SSD_GROUPS * SSD_STATE))
    dt = jax.nn.softplus((dt_raw + dt_bias).astype(jnp.float32))
    A = -jnp.exp(a_log.astype(jnp.float32))
    y = ssd_chunked(xs.reshape(b, L, SSD_HEADS, SSD_HD), dt, A,
                    bm.reshape(b, L, SSD_GROUPS, SSD_STATE),
                    cm.reshape(b, L, SSD_GROUPS, SSD_STATE), d_skip.astype(jnp.float32))
    y = y.reshape(b, L, SSD_W) * jax.nn.silu(z.astype(jnp.float32))
    y = rms_norm(y.reshape(b, L, SSD_GROUPS, SSD_W // SSD_GROUPS),
                 ssd_norm_w.reshape(SSD_GROUPS, SSD_W // SSD_GROUPS)).reshape(b, L, SSD_W)
    cat = jnp.concatenate([y_fox.astype(h.dtype), y.astype(h.dtype)], axis=-1)
    return cat @ w_out


def mixer_sb_gla(h, w_in, gate_w2, gate_b, gla_norm_w, w_out):
    b, L, _ = h.shape
    proj = h @ w_in
    sq, sk, sv, gq, gk, gv, g_low, gr = _split(proj, CD_SPLITS)
    y_sb = _from_heads(stick_breaking_attention(_to_heads(sq, SB_HEADS), _to_heads(sk, SB_HEADS),
                                                _to_heads(sv, SB_HEADS)))
    gate_logits = (g_low @ gate_w2 + gate_b).astype(jnp.float32).reshape(b, L, GLA_HEADS, GLA_DK)
    log_a = jax.nn.log_sigmoid(gate_logits) / GLA_GATE_NORM
    o = gla_chunked(gq.reshape(b, L, GLA_HEADS, GLA_DK) * (GLA_DK ** -0.5),
                    gk.reshape(b, L, GLA_HEADS, GLA_DK),
                    gv.reshape(b, L, GLA_HEADS, GLA_DV), log_a)
    o = rms_norm(o, gla_norm_w).reshape(b, L, GLA_HEADS * GLA_DV) * jax.nn.silu(gr)
    cat = jnp.concatenate([y_sb.astype(h.dtype), o.astype(h.dtype)], axis=-1)
    return cat @ w_out


def squared_relu_mlp(h, w_up, w_down):
    u = jax.nn.relu(h @ w_up)
    return (u * u) @ w_down


def setup_inputs(seed: int = 0) -> dict:
    key = jax.random.key(seed)
    ks = jax.random.split(key, 20)
    f32 = jnp.float32

    def nrm(k, shape, fan):
        return jax.random.normal(k, shape, f32) * (fan ** -0.5)

    def gain(k, shape):
        return 1.0 + 0.02 * jax.random.normal(k, shape, f32)

    x = jax.random.normal(ks[0], (BATCH, SEQ, D_MODEL), f32)
    norm_mix = gain(ks[1], (DEPTH, D_MODEL))
    norm_mlp = gain(ks[2], (DEPTH, D_MODEL))
    norm_final = gain(ks[3], (D_MODEL,))
    w_in_ab = nrm(ks[4], (N_EVEN, D_MODEL, AB_IN), D_MODEL)
    fox_f_bias = jax.random.uniform(ks[5], (N_EVEN, FOX_HEADS), f32, 1.0, 4.0)
    ssd_conv_w = nrm(ks[6], (N_EVEN, SSD_CONV, SSD_CONV_DIM), SSD_CONV)
    ssd_conv_b = 0.02 * jax.random.normal(ks[7], (N_EVEN, SSD_CONV_DIM), f32)
    dt0 = jnp.exp(jax.random.uniform(ks[8], (N_EVEN, SSD_HEADS), f32,
                                     math.log(1e-3), math.log(1e-1)))
    ssd_dt_bias = dt0 + jnp.log(-jnp.expm1(-dt0))
    ssd_a_log = jnp.log(jax.random.uniform(ks[9], (N_EVEN, SSD_HEADS), f32, 1.0, 16.0))
    ssd_d = gain(ks[10], (N_EVEN, SSD_HEADS))
    ssd_norm = gain(ks[11], (N_EVEN, SSD_W))
    w_out_ab = nrm(ks[12], (N_EVEN, MIX_W, D_MODEL), MIX_W)
    w_in_cd = nrm(ks[13], (N_ODD, D_MODEL, CD_IN), D_MODEL)
    gla_gate_w2 = nrm(ks[14], (N_ODD, GLA_RANK, GLA_HEADS * GLA_DK), GLA_RANK)
    gla_gate_b = 0.02 * jax.random.normal(ks[15], (N_ODD, GLA_HEADS * GLA_DK), f32)
    gla_norm = gain(ks[16], (N_ODD, GLA_DV))
    w_out_cd = nrm(ks[17], (N_ODD, MIX_W, D_MODEL), MIX_W)
    w_mlp_up = nrm(ks[18], (DEPTH, D_MODEL, D_FF), D_MODEL)
    w_mlp_down = nrm(ks[19], (DEPTH, D_FF, D_MODEL), D_FF)
    return {'x': x, 'norm_mix': norm_mix, 'norm_mlp': norm_mlp, 'norm_final': norm_final,
            'w_in_ab': w_in_ab, 'fox_f_bias': fox_f_bias, 'ssd_conv_w': ssd_conv_w,
            'ssd_conv_b': ssd_conv_b, 'ssd_dt_bias': ssd_dt_bias, 'ssd_a_log': ssd_a_log,
            'ssd_d': ssd_d, 'ssd_norm': ssd_norm, 'w_out_ab': w_out_ab,
            'w_in_cd': w_in_cd, 'gla_gate_w2': gla_gate_w2, 'gla_gate_b': gla_gate_b,
            'gla_norm': gla_norm, 'w_out_cd': w_out_cd,
            'w_mlp_up': w_mlp_up, 'w_mlp_down': w_mlp_down}


def reference(x, norm_mix, norm_mlp, norm_final, w_in_ab, fox_f_bias, ssd_conv_w, ssd_conv_b,
              ssd_dt_bias, ssd_a_log, ssd_d, ssd_norm, w_out_ab, w_in_cd, gla_gate_w2,
              gla_gate_b, gla_norm, w_out_cd, w_mlp_up, w_mlp_down):
    h = x
    for i in range(DEPTH):
        j = i // 2
        u = rms_norm(h, norm_mix[i])
        if i % 2 == 0:
            mix = mixer_fox_ssd(u, w_in_ab[j], fox_f_bias[j], ssd_conv_w[j], ssd_conv_b[j],
                                ssd_dt_bias[j], ssd_a_log[j], ssd_d[j], ssd_norm[j], w_out_ab[j])
        else:
            mix = mixer_sb_gla(u, w_in_cd[j], gla_gate_w2[j], gla_gate_b[j], gla_norm[j],
                               w_out_cd[j])
        h = h + mix.astype(h.dtype)
        u = rms_norm(h, norm_mlp[i])
        h = h + squared_relu_mlp(u, w_mlp_up[i], w_mlp_down[i]).astype(h.dtype)
    return rms_norm(h, norm_final)
```

```python
import contextlib
import numpy as np
import ml_dtypes
import concourse.bass as bass
import concourse.mybir as mybir
from concourse.bass_utils import run_bass_kernel_spmd

F32 = mybir.dt.float32
BF16 = mybir.dt.bfloat16
AF = mybir.ActivationFunctionType
ALU = mybir.AluOpType

SEM_CAP = 8000
N_DMA_SEMS = 24
TT = 512
SEQ = 8192
NEG = -30000.0


class Buf:
    def __init__(self, name, t, n=1):
        self.name = name
        self.t = t
        self.n = n
        self.lw = [None] * n
        self.rd = [[] for _ in range(n)]

    def __getitem__(self, k):
        return self.t[k]


class Op:
    __slots__ = ("eng", "fn", "deps", "dma", "signal", "skey", "sidx", "waits", "snap", "inc")

    def __init__(self, eng, fn, dma):
        self.eng = eng
        self.fn = fn
        self.dma = dma
        self.deps = {}
        self.signal = False
        self.skey = None
        self.sidx = 0
        self.waits = []
        self.snap = None
        self.inc = None


def _slots(b, s):
    if s is None:
        return range(b.n)
    if isinstance(s, int):
        return (s,)
    return s


class Sched:
    def __init__(self):
        self.ops = []
        self.dma_rr = 0
        self.dma_last = [None] * N_DMA_SEMS

    def add(self, eng, fn, reads=(), writes=(), dma=False):
        i = len(self.ops)
        op = Op(eng, fn, dma)
        for item in reads:
            b, s = item if isinstance(item, tuple) else (item, None)
            for k in _slots(b, s):
                if b.lw[k] is not None:
                    op.deps[b.lw[k]] = "RAW"
                b.rd[k].append(i)
        for item in writes:
            b, s = item if isinstance(item, tuple) else (item, None)
            for k in _slots(b, s):
                if b.lw[k] is not None and b.lw[k] != i:
                    op.deps.setdefault(b.lw[k], "WAW")
                for r in b.rd[k]:
                    if r != i:
                        op.deps.setdefault(r, "WAR")
                b.lw[k] = i
                b.rd[k] = []
        if dma:
            k = self.dma_rr % N_DMA_SEMS
            self.dma_rr += 1
            op.skey = ("dma", k)
            prev = self.dma_last[k]
            if prev is not None:
                op.deps.setdefault(prev, "SEM")
            self.dma_last[k] = i
        else:
            op.skey = eng
        for p in list(op.deps):
            po = self.ops[p]
            kind = op.deps[p]
            if not po.dma and not dma and po.eng == eng:
                if eng == "pe" or kind == "WAR":
                    del op.deps[p]
                    continue
            po.signal = True
        self.ops.append(op)
        return i

    def finalize(self, sems_alloc):
        counters = {}
        known = {}
        semh = {}

        def sem_for(skey, idx):
            if isinstance(skey, tuple):
                name = ("dma", skey[1])
                val = idx * 16
            else:
                g = (idx - 1) // SEM_CAP
                name = (skey, g)
                val = idx - g * SEM_CAP
            if name not in semh:
                semh[name] = sems_alloc("s_" + "_".join(str(x) for x in name))
            return semh[name], val

        for op in self.ops:
            kn = known.setdefault(op.eng, {})
            for p in sorted(op.deps):
                po = self.ops[p]
                if kn.get(po.skey, 0) >= po.sidx:
                    continue
                op.waits.append(sem_for(po.skey, po.sidx))
                kn[po.skey] = po.sidx
                for k2, v2 in po.snap.items():
                    if kn.get(k2, 0) < v2:
                        kn[k2] = v2
            if op.signal or op.dma:
                c = counters.get(op.skey, 0) + 1
                counters[op.skey] = c
                op.sidx = c
                op.inc = sem_for(op.skey, c)
                op.signal = True
            op.snap = dict(kn)

    def emit(self, engines):
        by_eng = {}
        for op in self.ops:
            by_eng.setdefault(op.eng, []).append(op)

        def make(engname):
            def body(e):
                for op in by_eng.get(engname, []):
                    for (sem, val) in op.waits:
                        e.wait_ge(sem, val)
                    ins = op.fn(e)
                    if op.signal:
                        ins.then_inc(op.inc[0], 16 if op.dma else 1)
            return body

        for engname, deco in engines.items():
            if by_eng.get(engname):
                deco(make(engname))


def build(nt, taps=(), stage=99):
    nc = bass.Bass("TRN2", target_bir_lowering=False)
    S = Sched()
    L = nt * TT
    NBT = 4 * nt

    def din(name, shape, dt=F32):
        return nc.dram_tensor(name, list(shape), dt, kind="ExternalInput").ap()

    xT = din("xT", [1024, L])
    w_in0 = din("w_in0", [1024, 2832])
    w_sm0 = din("w_sm0", [1024, 16])
    w_out0 = din("w_out0", [1024, 1024])
    w_in1 = din("w_in1", [1024, 3088])
    w_out1 = din("w_out1", [1024, 1024])
    w2aug = din("w2aug", [17, 256])
    smask = din("smask", [128, 4, 512])
    w_up = din("w_up", [2, 1024, 4096])
    w_dn = din("w_dn", [2, 4096, 1024])
    nrm = din("nrm", [128, 5, 8])
    rowp = din("rowp", [128, 2048])
    colp = din("colp", [128, 64])
    cmask = din("cmask", [128, 4, 512])
    tri = din("tri", [128, 4, 128])
    outT = nc.dram_tensor("outT", [1024, L], F32, kind="ExternalOutput").ap()
    KH = nc.dram_tensor("KH", [8, 65, L], BF16, kind="Internal").ap()
    wscr = {}
    for nm_, shp_ in (("w_in0", [1024, 2832]), ("w_sm0", [1024, 16]), ("w_out0", [1024, 1024]), ("w_in1", [1024, 3088]),
                      ("w_out1", [1024, 1024]), ("w_up", [2, 1024, 4096]), ("w_dn", [2, 4096, 1024])):
        wscr[nm_] = nc.dram_tensor(nm_ + "_bf", shp_, BF16, kind="Internal").ap()
    VH = nc.dram_tensor("VH", [L, 8, 65], BF16, kind="Internal").ap()
    KH1 = nc.dram_tensor("KH1", [8, 65, L], BF16, kind="Internal").ap()
    VH1 = nc.dram_tensor("VH1", [L, 8, 65], BF16, kind="Internal").ap()
    dbg = {}
    for (tname, shape) in taps:
        dbg[tname] = nc.dram_tensor("dbg_" + tname, list(shape), F32, kind="ExternalOutput").ap()

    es = contextlib.ExitStack()
    with es:
        def sb(name, shape, dt=F32, n=1):
            return Buf(name, es.enter_context(nc.sbuf_tensor(name, list(shape), dt)), n)

        PS = [Buf("ps%d" % i, es.enter_context(nc.psum_tensor("ps%d" % i, [128, 512], F32))) for i in range(8)]
        psc = [0]

        def psum():
            p = PS[psc[0] % 6]
            psc[0] += 1
            return p

        KHb = Buf("KH", KH, 8)
        VHb = Buf("VH", VH, 1)
        KH1b = Buf("KH1", KH1, 8)
        VH1b = Buf("VH1", VH1, 1)
        OUTb = Buf("outT", outT, 1)
        DBGb = Buf("dbg", None, 1)

        def dma(fn, reads=(), writes=(), q="sync"):
            S.add(q, fn, reads, writes, dma=True)

        def pe(fn, reads, writes):
            S.add("pe", fn, reads, writes)

        def act(fn, reads, writes):
            S.add("act", fn, reads, writes)

        def dve(fn, reads, writes):
            S.add("dve", fn, reads, writes)

        def pool(fn, reads, writes):
            S.add("pool", fn, reads, writes)

        hT = sb("hT", [128, 8, TT])
        uT = sb("uT", [128, 8, TT], BF16)
        sq = [sb("sq%d" % i, [128, TT], BF16) for i in range(2)]
        rstd = sb("rstd", [128, TT])
        ones_bf = sb("ones_bf", [128, 128], BF16)
        ones32 = sb("ones32", [128, 128])
        nrm_sb = sb("nrm_sb", [128, 5, 8])
        rowp_sb = sb("rowp_sb", [128, 2048])
        colp_sb = sb("colp_sb", [128, 64])
        cmask_bf = sb("cmask_bf", [128, 4, 512], BF16)
        tri_sb = sb("tri_sb", [128, 4, 128])
        wbf = [sb("wbf%d" % i, [128, 4096], BF16) for i in range(2)]
        wcnt = [0]
        QT = sb("QT", [65, 8, TT], BF16, n=8)
        KTc = sb("KTc", [65, 8, TT], BF16)
        Vc = sb("Vc", [128, 4, 8, 65], BF16)
        sm = sb("sm", [128, 4, 16])
        FH = sb("FH", [128, SEQ // 128, 8])
        Fcar = sb("Fcar", [128, 8])
        fx = sb("fx", [128, 4, 8])
        fsp = sb("fsp", [128, 4, 8])
        frel = sb("frel", [128, 4, 8])
        kbias = sb("kbias", [128, SEQ // 128, 8])
        F8T = sb("F8T", [8, TT], BF16)
        catH = sb("catH", [64, 16, TT], BF16, n=16)
        KTs = [sb("KTs%d" % i, [65, 2048], BF16) for i in range(2)]
        Vs = [sb("Vs%d" % i, [128, 16, 65], BF16) for i in range(2)]
        PT = [sb("PT%d" % i, [128, TT], BF16) for i in range(2)]
        lgt = [sb("lgt%d" % i, [128, TT]) for i in range(2)]
        mlpa = [sb("mlpa0", [128, 4, TT], BF16)] * 2

        dma(lambda e: e.dma_start(out=nrm_sb[:], in_=nrm), writes=[nrm_sb])
        dma(lambda e: e.dma_start(out=rowp_sb[:], in_=rowp), writes=[rowp_sb])
        dma(lambda e: e.dma_start(out=colp_sb[:], in_=colp), writes=[colp_sb])
        dma(lambda e: e.dma_start(out=tri_sb[:], in_=tri), writes=[tri_sb])
        pool(lambda e: e.memset(ones_bf[:], 1.0), [], [ones_bf])
        pool(lambda e: e.memset(ones32[:], 1.0), [], [ones32])
        pool(lambda e: e.memset(KTc[:], 1.0), [], [KTc])
        pool(lambda e: e.memset(Vc[:], 1.0), [], [Vc])
        pool(lambda e: e.memset(Fcar[:], 0.0), [], [Fcar])
        Uincl = tri_sb[:, 0, :]
        ident = tri_sb[:, 1, :]
        FB = rowp_sb[:, 0:8]

        def tap(name, buf, ap):
            if name in dbg:
                dma(lambda e: e.dma_start(out=dbg[name], in_=ap), reads=[buf], writes=[DBGb])

        WSb = Buf("wscr", None, 1)

        def load_w(src_ap, kc, ncols):
            i = wcnt[0] % 2
            wcnt[0] += 1
            bf = wbf[i]
            bfv = bf[:, 0:kc * ncols].rearrange("p (c n) -> p c n", c=kc)
            dma(lambda e: e.dma_start(out=bfv, in_=src_ap.rearrange("(c p) n -> p c n", p=128)), reads=[WSb], writes=[bf])
            return bf, bfv

        def rmsnorm(widx, dst, dst_dt_bf16=True):
            p = psum()
            for c in range(8):
                s = sq[c % 2]
                act(lambda e, c=c, s=s: e.activation(out=s[:], in_=hT[:, c, :], func=AF.Square), [hT], [s])
                pe(lambda e, c=c, s=s: e.matmul(p[:], ones_bf[:], s[:], start=(c == 0), stop=(c == 7)), [ones_bf, s], [p])
            act(lambda e: e.activation(out=rstd[:], in_=p[:], func=AF.Sqrt, bias=1e-5, scale=1.0 / 1024), [p], [rstd])
            dve(lambda e: e.reciprocal(out=rstd[:], in_=rstd[:]), [rstd], [rstd])
            for c in range(8):
                dve(lambda e, c=c: e.scalar_tensor_tensor(out=dst[:, c, :], in0=hT[:, c, :], scalar=nrm_sb[:, widx, c:c + 1],
                                                          in1=rstd[:], op0=ALU.mult, op1=ALU.mult), [hT, nrm_sb, rstd], [dst])

        def proj_fm(wb, wv, m0, M, evac):
            p = psum()
            for c in range(8):
                pe(lambda e, c=c: e.matmul(p[0:M, :], wv[:, c, m0:m0 + M], uT[:, c, :], start=(c == 0), stop=(c == 7)), [wb, uT], [p])
            evac(p)

        def proj_tm(wb, wv, n0, N, blk, p, pslice):
            for c in range(8):
                pe(lambda e, c=c: e.matmul(pslice, uT[:, c, blk * 128:(blk + 1) * 128], wv[:, c, n0:n0 + N], start=(c == 0), stop=(c == 7)), [wb, uT], [p])

        def mlp(layer):
            rmsnorm(1 + 2 * layer, uT)
            for g in range(8):
                wb, wv = load_w(w_up[layer, :, g * 512:(g + 1) * 512], 8, 512)
                a = mlpa[g % 2]
                for hc in range(4):
                    def ev(p, hc=hc, a=a):
                        act(lambda e: e.activation(out=lgt[0][:], in_=p[:], func=AF.Copy), [p], [lgt[0]])
                        dve(lambda e: e.scalar_tensor_tensor(out=a[:, hc, :], in0=lgt[0][:], scalar=0.0, in1=lgt[0][:], op0=ALU.max, op1=ALU.mult), [lgt[0]], [a])
                    proj_fm(wb, wv, hc * 128, 128, ev)
                import os
                if os.environ.get('MLPSKIP') == 'down':
                    continue
                for half in range(2):
                    wb2, wv2 = load_w(w_dn[layer, g * 512:(g + 1) * 512, half * 512:(half + 1) * 512], 4, 512)
                    for fc4 in range(4):
                        fc = half * 4 + fc4
                        p = psum()
                        for hc in range(4):
                            pe(lambda e, hc=hc, fc4=fc4, p=p, wv2=wv2, a=a: e.matmul(p[:], wv2[:, hc, fc4 * 128:(fc4 + 1) * 128], a[:, hc, :], start=(hc == 0), stop=(hc == 3)), [wb2, a], [p])
                        dve(lambda e, fc=fc, p=p: e.tensor_tensor(out=hT[:, fc, :], in0=hT[:, fc, :], in1=p[:], op=ALU.add), [hT, p], [hT])

        def out_proj(w_out):
            for colh in range(2):
                ps4 = [psum() for _ in range(4)]
                for ch in range(2):
                    i = wcnt[0] % 2
                    wcnt[0] += 1
                    bf = wbf[i]
                    bfv = bf[0:64, :].rearrange("p (c n) -> p c n", c=8)
                    src = w_out[ch * 512:(ch + 1) * 512, colh * 512:(colh + 1) * 512].rearrange("(c p) n -> p c n", p=64)
                    dma(lambda e, bfv=bfv, src=src: e.dma_start(out=bfv, in_=src), reads=[WSb], writes=[bf])
                    for fc in range(4):
                        for c in range(8):
                            cc = ch * 8 + c
                            pe(lambda e, fc=fc, c=c, cc=cc, bfv=bfv, ps4=ps4: e.matmul(ps4[fc][:], bfv[:, c, fc * 128:(fc + 1) * 128], catH[:, cc, :],
                                                                              start=(cc == 0), stop=(cc == 15)), [bf, (catH, cc)], [ps4[fc]])
                for fc in range(4):
                    f = colh * 4 + fc
                    dve(lambda e, f=f, fc=fc, ps4=ps4: e.tensor_tensor(out=hT[:, f, :], in0=hT[:, f, :], in1=ps4[fc][:], op=ALU.add), [hT, ps4[fc]], [hT])


        def MM(out, lhsT, rhs, start, stop, reads, writes):
            S.add("pe", lambda e: e.matmul(out, lhsT, rhs, start=start, stop=stop), reads, writes)

        def TR(out, in_, idn, reads, writes):
            S.add("pe", lambda e: e.transpose(out, in_, idn), reads, writes)

        def ACT(out, in_, func, reads, writes, bias=None, scale=None, accum_out=None):
            kw = {}
            if bias is not None:
                kw["bias"] = bias
            if scale is not None:
                kw["scale"] = scale
            if accum_out is not None:
                kw["accum_out"] = accum_out
            S.add("act", lambda e: e.activation(out=out, in_=in_, func=func, **kw), reads, writes)

        def TT_(out, in0, in1, op, reads, writes, eng="dve"):
            S.add(eng, lambda e: e.tensor_tensor(out=out, in0=in0, in1=in1, op=op), reads, writes)

        def STT(out, in0, scalar, in1, op0, op1, reads, writes, eng="dve"):
            S.add(eng, lambda e: e.scalar_tensor_tensor(out=out, in0=in0, scalar=scalar, in1=in1, op0=op0, op1=op1), reads, writes)

        def TS(out, in0, s1, s2, op0, op1, reads, writes, eng="dve"):
            if s2 is None:
                S.add(eng, lambda e: e.tensor_scalar(out=out, in0=in0, scalar1=s1, scalar2=None, op0=op0), reads, writes)
            else:
                S.add(eng, lambda e: e.tensor_scalar(out=out, in0=in0, scalar1=s1, scalar2=s2, op0=op0, op1=op1), reads, writes)

        def CP(out, in_, reads, writes, eng="dve"):
            S.add(eng, lambda e: e.tensor_copy(out=out, in_=in_), reads, writes)

        def MS(ap, val, writes, eng="pool"):
            S.add(eng, lambda e: e.memset(ap, val), [], writes)

        def RCP(out, in_, reads, writes):
            S.add("dve", lambda e: e.reciprocal(out=out, in_=in_), reads, writes)

        def DMA(out, in_, reads, writes):
            S.add("sync", lambda e: e.dma_start(out=out, in_=in_), reads, writes, dma=True)

        def proj_fm2(wb, wv, m0, M):
            p = psum()
            for c in range(8):
                MM(p[0:M, :], wv[:, c, m0:m0 + M], uT[:, c, :], c == 0, c == 7, [wb, uT], [p])
            return p

        def proj_tm2(wb, wv, n0, N, blk):
            p = psum()
            for c in range(8):
                MM(p[:, 0:N], uT[:, c, blk * 128:(blk + 1) * 128], wv[:, c, n0:n0 + N], c == 0, c == 7, [wb, uT], [p])
            return p

        zs = sb("zs", [128, 4, 512], BF16)
        xpre = sb("xpre", [128, 4, 515])
        bcpre = sb("bcpre", [64, 4, 515])
        halo_x = sb("halo_x", [128, 4, 3])
        halo_bc = sb("halo_bc", [64, 4, 3])
        acc = sb("acc", [128, 512])
        xcv = sb("xcv", [128, 512])
        rden = xcv
        outsb = acc
        x_tok = sb("x_tok", [128, 4, 512], BF16)
        B_tok = sb("B_tok", [128, 4, 128], BF16)
        BTb = sb("BTb", [64, 2, 512], BF16)
        CTb = sb("CTb", [64, 2, 512], BF16)
        dtb = sb("dtb", [128, 4, 8])
        av = sb("av", [128, 4, 8])
        acum = sb("acum", [128, 4, 8])
        Aneg = sb("Aneg", [128, 8])
        Xd = sb("Xd", [128, 8, 128])
        t1 = sb("t1", [128, 8, 128])
        Xe = sb("Xe", [128, 8, 128])
        cdb = sb("cdb", [128, 8])
        Etok = sb("Etok", [128, 8])
        Gs = sb("Gs", [128, 2, 128])
        Wt = sb("Wt", [128, 8, 128], BF16)
        xdt = sb("xdt", [128, 8, 64], BF16)
        Bdec = sb("Bdec", [128, 8, 64], BF16)
        Cdec = sb("Cdec", [64, 8, 128], BF16)
        dte = sb("dte", [128, 8])
        yv = sb("yv", [128, 512])
        yn = sb("yn", [128, 512])
        ssq = sb("ssq", [128, 2])
        Sst = sb("Sst", [64, 8, 64])
        Sbf = sb("Sbf", [64, 8, 64], BF16)
        MS(halo_x[:], 0.0, [halo_x])
        MS(halo_bc[:], 0.0, [halo_bc])
        MS(Sst[:], 0.0, [Sst])
        MS(Sbf[:], 0.0, [Sbf])
        Mneg = tri_sb[:, 2, :]
        DTB = rowp_sb[:, 8:16]
        ALOG = rowp_sb[:, 16:24]
        DSK = rowp_sb[:, 24:536]
        NWS = rowp_sb[:, 536:1048]
        ACT(Aneg[:], ALOG, AF.Exp, [rowp_sb], [Aneg])
        TS(Aneg[:], Aneg[:], -1.0, None, ALU.mult, None, [Aneg], [Aneg])

        import os
        SSDST = int(os.environ.get('SSDST', '99'))
        SSDSUB = int(os.environ.get('SSDSUB', '99'))

        def ssd_tile(ti):
            wb, wv = load_w(w_in0[:, 1544:2056], 8, 512)
            for b in range(4):
                p = proj_tm2(wb, wv, 0, 512, b)
                ACT(zs[:, b, :], p[:], AF.Silu, [p], [zs])
            wb, wv = load_w(w_in0[:, 2056:2568], 8, 512)
            CP(xpre[:, :, 0:3], halo_x[:], [halo_x], [xpre])
            for ch in range(4):
                p = proj_fm2(wb, wv, ch * 128, 128)
                ACT(xpre[:, ch, 3:515], p[:], AF.Copy, [p], [xpre])
            wb, wv = load_w(w_in0[:, 2568:2824], 8, 256)
            CP(bcpre[:, :, 0:3], halo_bc[:], [halo_bc], [bcpre])
            for j in range(4):
                p = proj_fm2(wb, wv, j * 64, 64)
                ACT(bcpre[:, j, 3:515], p[0:64, :], AF.Copy, [p], [bcpre])
            CP(halo_x[:], xpre[:, :, 512:515], [xpre], [halo_x])
            CP(halo_bc[:], bcpre[:, :, 512:515], [bcpre], [halo_bc])
            if SSDST < 2:
                return
            pxs = [psum() for _ in range(4)]
            for ch in range(4):
                TS(acc[:], xpre[:, ch, 0:512], colp_sb[:, ch * 4:ch * 4 + 1], None, ALU.mult, None, [xpre, colp_sb], [acc])
                for i in range(1, 4):
                    STT(acc[:], xpre[:, ch, i:i + 512], colp_sb[:, ch * 4 + i:ch * 4 + i + 1], acc[:], ALU.mult, ALU.add, [xpre, colp_sb, acc], [acc])
                ACT(xcv[:], acc[:], AF.Silu, [acc, colp_sb], [xcv], bias=colp_sb[:, 16 + ch:17 + ch])
                for b in range(4):
                    TR(pxs[b][:, ch * 128:(ch + 1) * 128], xcv[:, b * 128:(b + 1) * 128], ident, [xcv, tri_sb], [pxs[b]])
            for b in range(4):
                ACT(x_tok[:, b, :], pxs[b][:], AF.Copy, [pxs[b]], [x_tok])
            pbt = psum()
            for j in range(4):
                TS(acc[0:64, :], bcpre[:, j, 0:512], colp_sb[0:64, 20 + j * 4:21 + j * 4], None, ALU.mult, None, [bcpre, colp_sb], [acc])
                for i in range(1, 4):
                    STT(acc[0:64, :], bcpre[:, j, i:i + 512], colp_sb[0:64, 20 + j * 4 + i:21 + j * 4 + i], acc[0:64, :], ALU.mult, ALU.add, [bcpre, colp_sb, acc], [acc])
                ACT(xcv[0:64, :], acc[0:64, :], AF.Silu, [acc, colp_sb], [xcv], bias=colp_sb[0:64, 36 + j:37 + j])
                if j < 2:
                    CP(BTb[:, j, :], xcv[0:64, :], [xcv], [BTb])
                    for b in range(4):
                        TR(pbt[:, b * 128 + j * 64:b * 128 + (j + 1) * 64], xcv[0:64, b * 128:(b + 1) * 128], ident[0:64, 0:64], [xcv, tri_sb], [pbt])
                else:
                    CP(CTb[:, j - 2, :], xcv[0:64, :], [xcv], [CTb])
            ACT(B_tok[:].rearrange("p b n -> p (b n)"), pbt[:], AF.Copy, [pbt], [B_tok])
            if SSDST < 3:
                return
            TT_(dtb[:], sm[:, :, 8:16], DTB.unsqueeze(1).to_broadcast([128, 4, 8]), ALU.add, [sm, rowp_sb], [dtb])
            ACT(dtb[:], dtb[:], AF.Exp, [dtb], [dtb])
            ACT(dtb[:], dtb[:], AF.Ln, [dtb], [dtb], bias=1.0)
            TT_(av[:], dtb[:], Aneg[:].unsqueeze(1).to_broadcast([128, 4, 8]), ALU.mult, [dtb, Aneg], [av])
            pa = psum()
            for b in range(4):
                MM(pa[:, b * 8:(b + 1) * 8], Uincl, av[:, b, :], True, True, [tri_sb, av], [pa])
            CP(acum[:].rearrange("p b n -> p (b n)"), pa[:, 0:32], [pa], [acum])
            if SSDST < 4:
                return
            for b in range(4):
                blk = slice(b * 128, (b + 1) * 128)
                for h in range(8):
                    TS(Xd[:, h, :], ident, acum[:, b, h:h + 1], None, ALU.mult, None, [tri_sb, acum], [Xd])
                pab = [psum(), psum()]
                for k in range(2):
                    MM(pab[k][:], ones32[:], Xd[:, 4 * k:4 * k + 4, :].rearrange("p h l -> p (h l)"), True, True, [ones32, Xd], [pab[k]])
                if SSDSUB < 2:
                    continue
                pcl = psum()
                MM(pcl[:, 0:8], ones32[:], av[:, b, :], True, True, [ones32, av], [pcl])
                TT_(dte[:], pcl[:, 0:8], acum[:, b, :], ALU.subtract, [pcl, acum], [dte])
                ACT(dte[:], dte[:], AF.Exp, [dte], [dte])
                ACT(cdb[:], pcl[:, 0:8], AF.Exp, [pcl], [cdb])
                ACT(Etok[:], acum[:, b, :], AF.Exp, [acum], [Etok])
                for h in range(8):
                    TS(Xe[:, h, :], ident, Etok[:, h:h + 1], None, ALU.mult, None, [tri_sb, Etok], [Xe])
                pae = [psum(), psum()]
                for k in range(2):
                    MM(pae[k][:], ones32[:], Xe[:, 4 * k:4 * k + 4, :].rearrange("p h l -> p (h l)"), True, True, [ones32, Xe], [pae[k]])
                if SSDSUB < 3:
                    continue
                for k in range(2):
                    for hh in range(4):
                        h = 4 * k + hh
                        STT(t1[:, h, :], pab[k][:, hh * 128:(hh + 1) * 128], acum[:, b, h:h + 1], Mneg, ALU.subtract, ALU.min, [pab[k], acum, tri_sb], [t1])
                for k in range(2):
                    if True:
                        ACT(t1[:, 4 * k:4 * k + 4, :].rearrange("p h l -> p (h l)"), t1[:, 4 * k:4 * k + 4, :].rearrange("p h l -> p (h l)"), AF.Exp, [t1], [t1])
                if SSDSUB < 4:
                    continue
                pg = psum()
                for g in range(2):
                    MM(pg[:, g * 128:(g + 1) * 128], BTb[:, g, blk], CTb[:, g, blk], True, True, [BTb, CTb], [pg])
                ACT(Gs[:].rearrange("p g l -> p (g l)"), pg[:, 0:256], AF.Copy, [pg], [Gs])
                if SSDSUB < 5:
                    continue
                for g in range(2):
                    TT_(Wt[:, 4 * g:4 * g + 4, :], t1[:, 4 * g:4 * g + 4, :], Gs[:, g, :].unsqueeze(1).to_broadcast([128, 4, 128]), ALU.mult, [t1, Gs], [Wt])
                    TT_(Cdec[:, 4 * g:4 * g + 4, :], pae[g][0:64, :].rearrange("p (h l) -> p h l", h=4), CTb[:, g, blk].unsqueeze(1).to_broadcast([64, 4, 128]), ALU.mult, [pae[g], CTb], [Cdec])
                if SSDSUB < 6:
                    continue
                for h in range(8):
                    g = h // 4
                    TS(xdt[:, h, :], x_tok[:, b, h * 64:(h + 1) * 64], dtb[:, b, h:h + 1], None, ALU.mult, None, [x_tok, dtb], [xdt])
                    TS(Bdec[:, h, :], B_tok[:, b, g * 64:(g + 1) * 64], dte[:, h:h + 1], None, ALU.mult, None, [B_tok, dte], [Bdec])
                if SSDST < 5:
                    continue
                py = psum()
                for h in range(8):
                    MM(py[:, h * 64:(h + 1) * 64], Wt[:, h, :], xdt[:, h, :], True, False, [Wt, xdt], [py])
                    MM(py[:, h * 64:(h + 1) * 64], Cdec[:, h, :], Sbf[:, h, :], False, True, [Cdec, Sbf], [py])
                TT_(yv[:], x_tok[:, b, :], DSK, ALU.mult, [x_tok, rowp_sb], [yv])
                TT_(yv[:], yv[:], py[:], ALU.add, [yv, py], [yv])
                TT_(yv[:], yv[:], zs[:, b, :], ALU.mult, [yv, zs], [yv])
                MS(ssq[:], 0.0, [ssq], eng="dve")
                for g in range(2):
                    ACT(yn[:, g * 256:(g + 1) * 256], yv[:, g * 256:(g + 1) * 256], AF.Square, [yv], [yn, ssq], accum_out=ssq[:, g:g + 1])
                ACT(ssq[:], ssq[:], AF.Sqrt, [ssq], [ssq], bias=1e-5, scale=1.0 / 256)
                RCP(ssq[:], ssq[:], [ssq], [ssq])
                for g in range(2):
                    STT(yn[:, g * 256:(g + 1) * 256], yv[:, g * 256:(g + 1) * 256], ssq[:, g:g + 1], NWS[:, g * 256:(g + 1) * 256], ALU.mult, ALU.mult, [yv, ssq, rowp_sb], [yn])
                pT = [psum(), psum()]
                for h in range(8):
                    TR(pT[h // 4][0:64, (h % 4) * 128:(h % 4 + 1) * 128], yn[:, h * 64:(h + 1) * 64], ident, [yn, tri_sb], [pT[h // 4]])
                for k in range(2):
                    ACT(catH[:, 8 + 4 * k:12 + 4 * k, blk], pT[k][0:64, :].rearrange("p (h l) -> p h l", h=4), AF.Copy, [pT[k]], [(catH, s_) for s_ in range(8 + 4 * k, 12 + 4 * k)])
                if SSDST < 6:
                    continue
                pl = psum()
                for h in range(8):
                    MM(pl[0:64, h * 64:(h + 1) * 64], Bdec[:, h, :], xdt[:, h, :], True, True, [Bdec, xdt], [pl])
                for h in range(8):
                    STT(Sst[:, h, :], Sst[:, h, :], cdb[0:64, h:h + 1], pl[0:64, h * 64:(h + 1) * 64], ALU.mult, ALU.add, [Sst, cdb, pl], [Sst])
                ACT(Sbf[:], Sst[:], AF.Copy, [Sst], [Sbf])


        smask_bf = sb("smask_bf", [128, 4, 512], BF16)
        tri_bf = sb("tri_bf", [128, 2, 128], BF16)
        SG = sb("SG", [64, 4, 128])
        SGbf = sb("SGbf", [64, 4, 128], BF16)
        glT = sb("glT", [17, 512], BF16)
        W2b = sb("W2b", [17, 256], BF16)
        ssq4 = sb("ssq4", [128, 4])
        spb = [sb("spb%d" % i_, [128, TT]) for i_ in range(2)]
        hl = [sb("hl%d" % i_, [128, 2, TT], BF16) for i_ in range(2)]
        tlb = [sb("tlb%d" % i_, [128, TT]) for i_ in range(2)]
        for j_ in range(4):
            DMA(lgt[0][:], smask[:, j_, :], [], [lgt[0]])
            CP(smask_bf[:, j_, :], lgt[0][:], [lgt[0]], [smask_bf], eng="pool")
            DMA(lgt[1][:], cmask[:, j_, :], [], [lgt[1]])
            CP(cmask_bf[:, j_, :], lgt[1][:], [lgt[1]], [cmask_bf], eng="pool")
        CP(tri_bf[:, 0, :], tri_sb[:, 3, :], [tri_sb], [tri_bf], eng="pool")
        CP(tri_bf[:, 1, :], tri_sb[:, 1, :], [tri_sb], [tri_bf], eng="pool")
        DMA(lgt[1][0:17, 0:256], w2aug, [], [lgt[1]])
        CP(W2b[:], lgt[1][0:17, 0:256], [lgt[1]], [W2b], eng="pool")
        MS(SG[:], 0.0, [SG])
        MS(SGbf[:], 0.0, [SGbf])
        MS(glT[:], 1.0, [glT])
        Usuf_bf = tri_bf[:, 0, :]
        ident_bf = tri_bf[:, 1, :]
        GN = rowp_sb[:, 1048:1560]
        Rb = yv
        lrf = yn
        hi_t = mlpa[0]
        GQT = xpre
        GKT = bcpre
        gv = x_tok
        grs = zs
        gk_tok = Wt
        la = Xd
        gtok = Gs
        kdec = xdt
        tmpg = t1
        egb = Xe
        qkd = Cdec
        ATb = Bdec
        o_sb = yv
        on_ = yn
        GNR = acc

        def sb_tile(ti):
            t0 = ti * TT
            nkb = 4 * (ti + 1)
            wb, wv = load_w(w_in1[:, 0:512], 8, 512)
            for h in range(8):
                p = proj_fm2(wb, wv, h * 64, 64)
                ACT(QT[0:64, h, :], p[0:64, :], AF.Copy, [p], [(QT, h)], scale=0.125)
            wb, wv = load_w(w_in1[:, 512:1024], 8, 512)
            for h in range(8):
                p = proj_fm2(wb, wv, h * 64, 64)
                ACT(KTc[0:64, h, :], p[0:64, :], AF.Copy, [p], [KTc])
            for h in range(8):
                DMA(KH1[h, :, t0:t0 + TT], KTc[:, h, :], [KTc], [(KH1b, h)])
            wb, wv = load_w(w_in1[:, 1024:1536], 8, 512)
            for b in range(4):
                p = proj_tm2(wb, wv, 0, 512, b)
                ACT(Vc[:, b, :, 0:64], p[:].rearrange("p (h d) -> p h d", h=8), AF.Copy, [p], [Vc])
            DMA(VH1[t0:t0 + TT].rearrange("(b p) h d -> p b h d", p=128), Vc[:], [Vc], [VH1b])
            p_o = PS[6]
            for h in range(8):
                MS(Rb[:], 0.0, [Rb], eng="dve")
                nch = (nkb + 15) // 16
                blocks = []
                for kc in reversed(range(nch)):
                    nb = min(16, nkb - kc * 16)
                    for j in reversed(range(nb)):
                        blocks.append((kc, j, nb))
                st = {}
                loaded = {}

                def A1(i):
                    kc, j, nb = blocks[i]
                    if kc not in loaded:
                        k0 = kc * 2048
                        kt = KTs[(h + kc) % 2]
                        vs = Vs[(h + kc) % 2]
                        DMA(kt[:, 0:nb * 128], KH1[h, :, k0:k0 + nb * 128], [(KH1b, h)], [kt])
                        DMA(vs[:, 0:nb, :], VH1[k0:k0 + nb * 128, h, :].rearrange("(b p) d -> p b d", p=128), [VH1b], [vs])
                        loaded[kc] = (kt, vs)
                    kt, vs = loaded[kc]
                    kb = kc * 16 + j
                    dj = kb - 4 * ti
                    ksl = kt[0:64, j * 128:(j + 1) * 128]
                    pz = psum()
                    MM(pz[:], ksl, QT[0:64, h, :], True, dj < 0, [kt, (QT, h)], [pz])
                    if dj >= 0:
                        MM(pz[:], ident_bf, smask_bf[:, dj, :], False, True, [tri_bf, smask_bf], [pz])
                    st[i] = dict(kt=kt, vs=vs, kb=kb, dj=dj, ksl=ksl, pz=pz, par=kb % 2, j=j)

                def A2act(i):
                    d = st[i]
                    par = d["par"]
                    eb_, sp_, hl_ = lgt[par], spb[par], hl[par]
                    ACT(eb_[:], d["pz"][:], AF.Exp, [d["pz"]], [eb_])
                    ACT(sp_[:], eb_[:], AF.Ln, [eb_], [sp_], bias=1.0)
                    TS(hl_[:, 0, :], sp_[:], -1.0, None, ALU.mult, None, [sp_], [hl_], eng="pool")

                def A2dve(i):
                    d = st[i]
                    par = d["par"]
                    sp_, hl_ = spb[par], hl[par]
                    STT(hl_[:, 1, :], sp_[:], -1.0, hl_[:, 0, :], ALU.mult, ALU.subtract, [sp_, hl_], [hl_])

                def B_(i):
                    d = st[i]
                    hl_ = hl[d["par"]]
                    pL = psum()
                    MM(pL[:], d["ksl"], QT[0:64, h, :], True, False, [d["kt"], (QT, h)], [pL])
                    if d["dj"] >= 0:
                        MM(pL[:], ident_bf, smask_bf[:, d["dj"], :], False, False, [tri_bf, smask_bf], [pL])
                    MM(pL[:], Usuf_bf, hl_[:, 0, :], False, False, [tri_bf, hl_], [pL])
                    MM(pL[:], Usuf_bf, hl_[:, 1, :], False, True, [tri_bf, hl_], [pL])
                    pR = psum()
                    MM(pR[:], ones_bf[:], hl_[:, 0, :], True, False, [ones_bf, hl_], [pR])
                    MM(pR[:], ones_bf[:], hl_[:, 1, :], False, True, [ones_bf, hl_], [pR])
                    d["pL"] = pL
                    d["pR"] = pR

                def C1a(i):
                    d = st[i]
                    tl_ = tlb[d["par"]]
                    TT_(tl_[:], d["pL"][:], Rb[:], ALU.add, [d["pL"], Rb], [tl_])
                    TT_(Rb[:], Rb[:], d["pR"][:], ALU.add, [Rb, d["pR"]], [Rb])

                def C1b(i):
                    d = st[i]
                    tl_ = tlb[d["par"]]
                    ACT(PT[d["par"]][:], tl_[:], AF.Exp, [tl_], [PT[d["par"]]])

                def C2(i):
                    d = st[i]
                    MM(p_o[0:64, :], d["vs"][:, d["j"], 0:64], PT[d["par"]][:], d["kb"] == nkb - 1, d["kb"] == 0, [d["vs"], PT[d["par"]]], [p_o])
                    del st[i]

                nbk = len(blocks)
                A1(0)
                for i in range(nbk + 2):
                    if 0 <= i - 2 < nbk:
                        C1a(i - 2)
                    if i < nbk:
                        A2act(i)
                    if 0 <= i - 2 < nbk:
                        C1b(i - 2)
                    if i < nbk:
                        A2dve(i)
                    if 0 <= i - 1 < nbk:
                        B_(i - 1)
                    if i + 1 < nbk:
                        A1(i + 1)
                    if 0 <= i - 2 < nbk:
                        C2(i - 2)
                ACT(catH[:, h, :], p_o[0:64, :], AF.Copy, [p_o], [(catH, h)])

        def gla_tile(ti):
            wb, wv = load_w(w_in1[:, 1536:2048], 8, 512)
            for h in range(4):
                p = proj_fm2(wb, wv, h * 64, 64)
                ACT(GQT[0:64, h, 0:512], p[0:64, :], AF.Copy, [p], [GQT])
            for h in range(4):
                p = proj_fm2(wb, wv, 256 + h * 64, 64)
                ACT(GKT[0:64, h, 0:512], p[0:64, :], AF.Copy, [p], [GKT])
            gkv = gk_tok[:].rearrange("p (b x) l -> p b (x l)", b=4)
            for b in range(4):
                p = proj_tm2(wb, wv, 256, 256, b)
                ACT(gkv[:, b, :], p[:, 0:256], AF.Copy, [p], [gk_tok])
            wb, wv = load_w(w_in1[:, 2048:2560], 8, 512)
            for b in range(4):
                p = proj_tm2(wb, wv, 0, 512, b)
                ACT(gv[:, b, :], p[:], AF.Copy, [p], [gv])
            wb, wv = load_w(w_in1[:, 2576:3088], 8, 512)
            for b in range(4):
                p = proj_tm2(wb, wv, 0, 512, b)
                ACT(grs[:, b, :], p[:], AF.Silu, [p], [grs])
            wb, wv = load_w(w_in1[:, 2560:2576], 8, 16)
            p = proj_fm2(wb, wv, 0, 16)
            ACT(glT[0:16, :], p[0:16, :], AF.Copy, [p], [glT])
            lav = la[:].rearrange("p (b x) l -> p b (x l)", b=4)
            for b in range(4):
                blk = slice(b * 128, (b + 1) * 128)
                pgl = psum()
                MM(pgl[:, 0:256], glT[0:17, blk], W2b[0:17, :], True, True, [glT, W2b], [pgl])
                ACT(lav[:, b, :], pgl[:, 0:256], AF.Exp, [pgl], [la], scale=-1.0)
            for b in range(4):
                ACT(lav[:, b, :], lav[:, b, :], AF.Ln, [la], [la], bias=1.0)
                TS(lav[:, b, :], lav[:, b, :], -1.0 / 16.0, None, ALU.mult, None, [la], [la])
            gtv = gtok[:].rearrange("p g l -> p (g l)")
            tmv = tmpg[:].rearrange("p h l -> p (h l)")
            kdv = kdec[:].rearrange("p h d -> p (h d)")
            for b in range(4):
                blk = slice(b * 128, (b + 1) * 128)
                pgt = psum()
                MM(pgt[:, 0:256], Uincl, lav[:, b, :], True, True, [tri_sb, la], [pgt])
                pgs = psum()
                MM(pgs[:, 0:256], ones32[:], lav[:, b, :], True, True, [ones32, la], [pgs])
                CP(gtv, pgt[:, 0:256], [pgt], [gtok])
                TT_(tmv[:, 0:256], pgs[:, 0:256], gtv, ALU.subtract, [pgs, gtok], [tmpg])
                ACT(tmv[:, 0:256], tmv[:, 0:256], AF.Exp, [tmpg], [tmpg])
                TT_(kdv[:, 0:256], gkv[:, b, :], tmv[:, 0:256], ALU.mult, [gk_tok, tmpg], [kdec])
                pgT = psum()
                for h in range(4):
                    MM(pgT[0:64, h * 128:(h + 1) * 128], lav[:, b, h * 64:(h + 1) * 64], Uincl, True, True, [la, tri_sb], [pgT])
                CP(tmv[0:64, 512:1024], pgT[0:64, :], [pgT], [tmpg])
                egv = egb[0:64, 0:4, :].rearrange("p h l -> p (h l)")
                engv = egb[0:64, 4:8, :].rearrange("p h l -> p (h l)")
                ACT(egv, tmv[0:64, 512:1024], AF.Exp, [tmpg], [egb])
                ACT(engv, tmv[0:64, 512:1024], AF.Exp, [tmpg], [egb], scale=-1.0)
                STT(qkd[:, 0:4, :], GQT[0:64, :, blk], 0.125, egb[0:64, 0:4, :], ALU.mult, ALU.mult, [GQT, egb], [qkd])
                TT_(qkd[:, 4:8, :], GKT[0:64, :, blk], egb[0:64, 4:8, :], ALU.mult, [GKT, egb], [qkd])
                pA = psum()
                for h in range(4):
                    MM(pA[:, h * 128:(h + 1) * 128], qkd[:, 4 + h, :], qkd[:, h, :], True, True, [qkd], [pA])
                atv = ATb[:].rearrange("p (h x) d -> p h (x d)", h=4)
                TT_(atv, pA[:].rearrange("p (h l) -> p h l", h=4), Uincl.unsqueeze(1).to_broadcast([128, 4, 128]), ALU.mult, [pA, tri_sb], [ATb])
                po = psum()
                for h in range(4):
                    MM(po[:, h * 128:(h + 1) * 128], atv[:, h, :], gv[:, b, h * 128:(h + 1) * 128], True, False, [ATb, gv], [po])
                    MM(po[:, h * 128:(h + 1) * 128], qkd[:, h, :], SGbf[:, h, :], False, True, [qkd, SGbf], [po])
                CP(o_sb[:], po[:], [po], [o_sb])
                MS(ssq4[:], 0.0, [ssq4], eng="dve")
                for h in range(4):
                    ACT(on_[:, h * 128:(h + 1) * 128], o_sb[:, h * 128:(h + 1) * 128], AF.Square, [o_sb], [on_, ssq4], accum_out=ssq4[:, h:h + 1])
                ACT(ssq4[:], ssq4[:], AF.Sqrt, [ssq4], [ssq4], bias=1e-5, scale=1.0 / 128)
                RCP(ssq4[:], ssq4[:], [ssq4], [ssq4])
                TT_(GNR[:], grs[:, b, :], GN, ALU.mult, [grs, rowp_sb], [GNR])
                for h in range(4):
                    STT(on_[:, h * 128:(h + 1) * 128], o_sb[:, h * 128:(h + 1) * 128], ssq4[:, h:h + 1], GNR[:, h * 128:(h + 1) * 128], ALU.mult, ALU.mult, [o_sb, ssq4, GNR], [on_])
                pT = [psum(), psum()]
                for c in range(8):
                    TR(pT[c // 4][0:64, (c % 4) * 128:(c % 4 + 1) * 128], on_[:, c * 64:(c + 1) * 64], ident, [on_, tri_sb], [pT[c // 4]])
                for k in range(2):
                    ACT(catH[:, 8 + 4 * k:12 + 4 * k, blk], pT[k][0:64, :].rearrange("p (h l) -> p h l", h=4), AF.Copy, [pT[k]], [(catH, s_) for s_ in range(8 + 4 * k, 12 + 4 * k)])
                pl = psum()
                for h in range(4):
                    MM(pl[0:64, h * 128:(h + 1) * 128], kdv[:, h * 64:(h + 1) * 64], gv[:, b, h * 128:(h + 1) * 128], True, True, [kdec, gv], [pl])
                for h in range(4):
                    STT(SG[:, h, :], SG[:, h, :], egb[0:64, h, 127:128], pl[0:64, h * 128:(h + 1) * 128], ALU.mult, ALU.add, [SG, egb, pl], [SG])
                ACT(SGbf[:], SG[:], AF.Copy, [SG], [SGbf])

        hTf = hT[:].rearrange("p c t -> p (c t)")
        pieces = []
        for nm_, src_ in (("w_sm0", w_sm0), ("w_in0", w_in0), ("w_out0", w_out0), ("w_in1", w_in1), ("w_out1", w_out1)):
            R_, C_ = src_.shape
            for r0 in range(0, R_, 128):
                pieces.append((src_[r0:r0 + 128, :], wscr[nm_][r0:r0 + 128, :], C_))
        for l_ in range(2):
            for r0 in range(0, 1024, 128):
                pieces.append((w_up[l_, r0:r0 + 128, :], wscr["w_up"][l_, r0:r0 + 128, :], 4096))
            for r0 in range(0, 4096, 512):
                pieces.append((w_dn[l_, r0:r0 + 512, :].rearrange("(a p) n -> p a n", p=128), wscr["w_dn"][l_, r0:r0 + 512, :].rearrange("(a p) n -> p a n", p=128), (4, 1024)))
        for pi_, (src_, dst_, C_) in enumerate(pieces):
            bfb = wbf[pi_ % 2]
            if isinstance(C_, tuple):
                a_, n_ = C_
                stv_ = hTf[:, 0:a_ * n_].rearrange("p (a n) -> p a n", a=a_)
                bfv_ = bfb[:, 0:a_ * n_].rearrange("p (a n) -> p a n", a=a_)
                for a1 in range(a_):
                    for hf in range(2):
                        DMA(stv_[:, a1, hf * 512:(hf + 1) * 512], src_[:, a1, hf * 512:(hf + 1) * 512], [], [hT])
                tot_ = a_ * n_
            else:
                stv_ = hTf[:, 0:C_]
                bfv_ = bfb[:, 0:C_]
                for c0 in range(0, C_, 512):
                    c1 = min(C_, c0 + 512)
                    DMA(stv_[:, c0:c1], src_[:, c0:c1], [], [hT])
                tot_ = C_
            half_ = (tot_ + 1) // 2
            CP(bfb[:, 0:half_], hTf[:, 0:half_], [hT], [bfb], eng="dve")
            if tot_ > half_:
                ACT(bfb[:, half_:tot_], hTf[:, half_:tot_], AF.Copy, [hT], [bfb])
            if isinstance(C_, tuple):
                for a1 in range(a_):
                    DMA(dst_[:, a1, :], bfv_[:, a1, :], [bfb], [WSb])
            else:
                DMA(dst_, bfv_, [bfb], [WSb])
        w_sm0 = wscr["w_sm0"]
        w_in0 = wscr["w_in0"]
        w_out0 = wscr["w_out0"]
        w_in1 = wscr["w_in1"]
        w_out1 = wscr["w_out1"]
        w_up = wscr["w_up"]
        w_dn = wscr["w_dn"]

        for ti in range(nt):
            t0 = ti * TT
            dma(lambda e, t0=t0: e.dma_start(out=hT[:], in_=xT[:, t0:t0 + TT].rearrange("(c p) t -> p c t", p=128)), writes=[hT])
            rmsnorm(0, uT)
            wb, wv = load_w(w_sm0, 8, 16)
            p = psum()
            for b in range(4):
                proj_tm(wb, wv, 0, 16, b, p, p[:, b * 16:(b + 1) * 16])
            act(lambda e, p=p: e.activation(out=sm[:].rearrange("p b n -> p (b n)"), in_=p[:, 0:64], func=AF.Copy), [p], [sm])
            dve(lambda e: e.tensor_tensor(out=fx[:], in0=sm[:, :, 0:8], in1=FB.unsqueeze(1).to_broadcast([128, 4, 8]), op=ALU.add), [sm, rowp_sb], [fx])
            act(lambda e: e.activation(out=fx[:], in_=fx[:], func=AF.Exp, scale=-1.0), [fx], [fx])
            act(lambda e: e.activation(out=fsp[:], in_=fx[:], func=AF.Ln, bias=1.0), [fx], [fsp])
            pc = psum()
            pcs = psum()
            for b in range(4):
                pe(lambda e, b=b, pc=pc, pcs=pcs: e.matmul(pc[:, b * 8:(b + 1) * 8], Uincl, fsp[:, b, :], start=True, stop=True), [tri_sb, fsp], [pc])
                pe(lambda e, b=b, pc=pc, pcs=pcs: e.matmul(pcs[:, b * 8:(b + 1) * 8], ones32[:], fsp[:, b, :], start=True, stop=True), [ones32, fsp], [pcs])
            for b in range(4):
                if b == 0:
                    dve(lambda e, pc=pc, pcs=pcs: e.tensor_copy(out=frel[:, 0, :], in_=pc[:, 0:8]), [pc], [frel])
                else:
                    dve(lambda e, b=b, pc=pc, pcs=pcs: e.tensor_tensor(out=frel[:, b, :], in0=pc[:, b * 8:(b + 1) * 8], in1=fx[:, b, :], op=ALU.add), [pc, fx], [frel])
                if b < 3:
                    if b == 0:
                        dve(lambda e, pc=pc, pcs=pcs: e.tensor_copy(out=fx[:, 1, :], in_=pcs[:, 0:8]), [pcs], [fx])
                    else:
                        dve(lambda e, b=b, pc=pc, pcs=pcs: e.tensor_tensor(out=fx[:, b + 1, :], in0=fx[:, b, :], in1=pcs[:, b * 8:(b + 1) * 8], op=ALU.add), [pcs, fx], [fx])
            dve(lambda e, ti=ti: e.tensor_tensor(out=FH[:, ti * 4:(ti + 1) * 4, :], in0=frel[:], in1=Fcar[:].unsqueeze(1).to_broadcast([128, 4, 8]), op=ALU.add),
                [frel, Fcar], [FH])
            nkb = 4 * (ti + 1)
            dve(lambda e, nkb=nkb: e.tensor_tensor(out=kbias[:, 0:nkb, :], in0=FH[:, 0:nkb, :], in1=Fcar[:].unsqueeze(1).to_broadcast([128, nkb, 8]), op=ALU.subtract),
                [FH, Fcar], [kbias])
            pt = psum()
            for b in range(4):
                pe(lambda e, b=b, pt=pt: e.transpose(pt[0:8, b * 128:(b + 1) * 128], frel[:, b, :], ident), [frel, tri_sb], [pt])
            act(lambda e, pt=pt: e.activation(out=F8T[:], in_=pt[0:8, :], func=AF.Copy, scale=-8.0), [pt], [F8T])
            dve(lambda e, pc=pc, pcs=pcs: e.tensor_tensor(out=fx[:, 0, :], in0=fx[:, 3, :], in1=pcs[:, 24:32], op=ALU.add), [fx, pcs], [fx])
            dve(lambda e: e.tensor_tensor(out=Fcar[:], in0=Fcar[:], in1=fx[:, 0, :], op=ALU.add), [fx, Fcar], [Fcar])
            if stage < 2:
                break
            if stage == 77:
                mlp(0)
                break
            wb, wv = load_w(w_in0[:, 0:512], 8, 512)
            for h in range(8):
                proj_fm(wb, wv, h * 64, 64, lambda p, h=h: act(lambda e: e.activation(out=QT[0:64, h, :], in_=p[0:64, :], func=AF.Copy), [p], [(QT, h)]))
            for h in range(8):
                dma(lambda e, h=h: e.dma_start(out=QT[64:65, h, :], in_=F8T[h:h + 1, :]), reads=[F8T], writes=[(QT, h)])
            wb, wv = load_w(w_in0[:, 512:1024], 8, 512)
            for h in range(8):
                proj_fm(wb, wv, h * 64, 64, lambda p, h=h: act(lambda e: e.activation(out=KTc[0:64, h, :], in_=p[0:64, :], func=AF.Copy), [p], [KTc]))
            for h in range(8):
                dma(lambda e, h=h, t0=t0: e.dma_start(out=KH[h, :, t0:t0 + TT], in_=KTc[:, h, :]), reads=[KTc], writes=[(KHb, h)])
            wb, wv = load_w(w_in0[:, 1024:1536], 8, 512)
            for b in range(4):
                p = psum()
                proj_tm(wb, wv, 0, 512, b, p, p[:])
                act(lambda e, b=b, p=p: e.activation(out=Vc[:, b, :, 0:64], in_=p[:].rearrange("p (h d) -> p h d", h=8), func=AF.Copy), [p], [Vc])
            dma(lambda e, t0=t0: e.dma_start(out=VH[t0:t0 + TT].rearrange("(b p) h d -> p b h d", p=128), in_=Vc[:]), reads=[Vc], writes=[VHb])
            if stage < 3:
                break
            for h in range(8):
                p_o = PS[6]
                p_d = PS[7]
                pend = None
                for kc in range((nkb + 15) // 16):
                    k0 = kc * 2048
                    nb = min(16, nkb - kc * 16)
                    kt = KTs[(h + kc) % 2]
                    vs = Vs[(h + kc) % 2]
                    DMA(kt[:, 0:nb * 128], KH[h, :, k0:k0 + nb * 128], [(KHb, h)], [kt])
                    DMA(vs[:, 0:nb, :], VH[k0:k0 + nb * 128, h, :].rearrange("(b p) d -> p b d", p=128), [VHb], [vs])
                    for j in range(nb):
                        kb = kc * 16 + j
                        dj = kb - 4 * ti
                        ps_ = psum()
                        MM(ps_[:], kt[:, j * 128:(j + 1) * 128], QT[:, h, :], True, dj < 0, [kt, (QT, h)], [ps_])
                        if dj >= 0:
                            MM(ps_[:], tri_bf[:, 1, :], cmask_bf[:, dj, :], False, True, [tri_bf, cmask_bf], [ps_])
                        ptile = PT[kb % 2]
                        ACT(ptile[:], ps_[:], AF.Exp, [ps_, kbias], [ptile], bias=kbias[:, kb, h:h + 1], scale=0.125)

                        def st2(vs=vs, j=j, ptile=ptile, kb=kb):
                            MM(p_o[0:64, :], vs[:, j, 0:64], ptile[:], kb == 0, kb == nkb - 1, [vs, ptile], [p_o])
                            MM(p_d[0:64, :], ones_bf[:, 0:64], ptile[:], kb == 0, kb == nkb - 1, [ones_bf, ptile], [p_d])
                        if pend is not None:
                            pend()
                        pend = st2
                pend()
                dve(lambda e, p_d=p_d: e.reciprocal(out=rden[0:64, :], in_=p_d[0:64, :]), [p_d], [rden])
                dve(lambda e, h=h, p_o=p_o: e.tensor_tensor(out=catH[:, h, :], in0=p_o[0:64, :], in1=rden[0:64, :], op=ALU.mult), [p_o, rden], [(catH, h)])
            if stage < 4:
                break
            ssd_tile(ti)
            out_proj(w_out0)
            if stage >= 5:
                mlp(0)
            if stage >= 6:
                rmsnorm(2, uT)
                sb_tile(ti)
                if stage >= 7:
                    gla_tile(ti)
                else:
                    for h_ in range(8, 16):
                        MS(catH[:, h_, :], 0.0, [(catH, h_)])
                out_proj(w_out1)
                if stage >= 8:
                    mlp(1)
            p = psum()
            for c in range(8):
                s = sq[c % 2]
                act(lambda e, c=c, s=s: e.activation(out=s[:], in_=hT[:, c, :], func=AF.Square), [hT], [s])
                pe(lambda e, c=c, s=s, p=p: e.matmul(p[:], ones_bf[:], s[:], start=(c == 0), stop=(c == 7)), [ones_bf, s], [p])
            act(lambda e, p=p: e.activation(out=rstd[:], in_=p[:], func=AF.Sqrt, bias=1e-5, scale=1.0 / 1024), [p], [rstd])
            dve(lambda e: e.reciprocal(out=rstd[:], in_=rstd[:]), [rstd], [rstd])
            for c in range(8):
                dve(lambda e, c=c: e.scalar_tensor_tensor(out=outsb[:], in0=hT[:, c, :], scalar=nrm_sb[:, 4, c:c + 1], in1=rstd[:], op0=ALU.mult, op1=ALU.mult),
                    [hT, nrm_sb, rstd], [outsb])
                dma(lambda e, c=c, t0=t0: e.dma_start(out=outT[c * 128:(c + 1) * 128, t0:t0 + TT], in_=outsb[:]), reads=[outsb], writes=[OUTb])
        S.add("sync", lambda e: e.nop(), reads=[OUTb, DBGb])
        S.finalize(lambda name: es.enter_context(nc.semaphore(name)))
        block = es.enter_context(nc.Block())
        S.emit({"sync": block.sync, "act": block.scalar, "dve": block.vector, "pool": block.gpsimd, "pe": block.tensor})
    return nc


def prep_inputs(inp, b, nt):
    L = nt * TT
    f = np.float32
    d = {}
    d["xT"] = np.ascontiguousarray(inp["x"][b, :L, :].T).astype(f)
    w0 = np.asarray(inp["w_in_ab"][0], f)
    d["w_in0"] = np.ascontiguousarray(w0)
    d["w_sm0"] = np.ascontiguousarray(np.concatenate([w0[:, 1536:1544], w0[:, 2824:2832]], axis=1))
    d["w_out0"] = np.ascontiguousarray(inp["w_out_ab"][0], f)
    d["w_in1"] = np.ascontiguousarray(inp["w_in_cd"][0], f)
    d["w_out1"] = np.ascontiguousarray(inp["w_out_cd"][0], f)
    d["w2aug"] = np.ascontiguousarray(np.concatenate([inp["gla_gate_w2"][0], inp["gla_gate_b"][0][None, :]], 0), f)
    d["w_up"] = np.ascontiguousarray(inp["w_mlp_up"], f)
    d["w_dn"] = np.ascontiguousarray(inp["w_mlp_down"], f)
    nr = np.stack([inp["norm_mix"][0], inp["norm_mlp"][0], inp["norm_mix"][1], inp["norm_mlp"][1], inp["norm_final"]], 0).astype(f)
    d["nrm"] = np.ascontiguousarray(nr.reshape(5, 8, 128).transpose(2, 0, 1))
    rowp = np.zeros((2048,), f)
    rowp[0:8] = inp["fox_f_bias"][0]
    rowp[8:16] = inp["ssd_dt_bias"][0]
    rowp[16:24] = inp["ssd_a_log"][0]
    rowp[24:536] = np.repeat(inp["ssd_d"][0], 64)
    rowp[536:1048] = inp["ssd_norm"][0]
    rowp[1048:1560] = np.tile(inp["gla_norm"][0], 4)
    d["rowp"] = np.ascontiguousarray(np.broadcast_to(rowp[None, :], (128, 2048)))
    colp = np.zeros((128, 64), f)
    cw = np.asarray(inp["ssd_conv_w"][0], f)
    cb = np.asarray(inp["ssd_conv_b"][0], f)
    for ch in range(4):
        colp[:, ch * 4:ch * 4 + 4] = cw[:, ch * 128:(ch + 1) * 128].T
        colp[:, 16 + ch] = cb[ch * 128:(ch + 1) * 128]
    for j in range(4):
        colp[0:64, 20 + j * 4:24 + j * 4] = cw[:, 512 + j * 64:512 + (j + 1) * 64].T
        colp[0:64, 36 + j] = cb[512 + j * 64:512 + (j + 1) * 64]
    d["colp"] = colp
    cm = np.zeros((128, 4, 512), f)
    pp = np.arange(128)[:, None]
    ii = np.arange(512)[None, :]
    for j in range(4):
        cm[:, j, :] = np.where(pp + 128 * j <= ii, 0.0, NEG)
    d["cmask"] = cm
    tri = np.zeros((128, 4, 128), f)
    tri[:, 0, :] = (pp <= np.arange(128)[None, :])
    tri[:, 1, :] = np.eye(128)
    tri[:, 2, :] = np.where(np.arange(128)[None, :] >= pp, 0.0, NEG)
    tri[:, 3, :] = (pp >= np.arange(128)[None, :])
    d["tri"] = tri
    sm_ = np.zeros((128, 4, 512), f)
    for j in range(4):
        sm_[:, j, :] = np.where(pp + 128 * j < ii, 0.0, NEG)
    d["smask"] = sm_
    return d


_CACHE = {}


def kernel(**inputs):
    inp = {k: np.asarray(v) for k, v in inputs.items()}
    nt = SEQ // TT
    if nt not in _CACHE:
        _CACHE[nt] = build(nt)
    nc = _CACHE[nt]
    in_maps = [prep_inputs(inp, b, nt) for b in range(2)]
    res = run_bass_kernel_spmd(nc, in_maps, core_ids=[0, 1])
    out = np.stack([res.results[b]["outT"].T for b in range(2)], 0)
    return np.ascontiguousarray(out).astype(np.float32)
```

```python
import contextlib
import numpy as np
import ml_dtypes
import concourse.bass as bass
import concourse.mybir as mybir
from concourse.bass_utils import run_bass_kernel_spmd

F32 = mybir.dt.float32
BF16 = mybir.dt.bfloat16
AF = mybir.ActivationFunctionType
ALU = mybir.AluOpType

SEM_CAP = 8000
N_DMA_SEMS = 24
TT = 512
SEQ = 8192
NEG = -30000.0


class Buf:
    def __init__(self, name, t, n=1):
        self.name = name
        self.t = t
        self.n = n
        self.lw = [None] * n
        self.rd = [[] for _ in range(n)]

    def __getitem__(self, k):
        return self.t[k]


class Op:
    __slots__ = ("eng", "fn", "deps", "dma", "signal", "skey", "sidx", "waits", "snap", "inc")

    def __init__(self, eng, fn, dma):
        self.eng = eng
        self.fn = fn
        self.dma = dma
        self.deps = {}
        self.signal = False
        self.skey = None
        self.sidx = 0
        self.waits = []
        self.snap = None
        self.inc = None


def _slots(b, s):
    if s is None:
        return range(b.n)
    if isinstance(s, int):
        return (s,)
    return s


class Sched:
    def __init__(self):
        self.ops = []
        self.dma_rr = 0
        self.dma_last = [None] * N_DMA_SEMS

    def add(self, eng, fn, reads=(), writes=(), dma=False):
        i = len(self.ops)
        op = Op(eng, fn, dma)
        for item in reads:
            b, s = item if isinstance(item, tuple) else (item, None)
            for k in _slots(b, s):
                if b.lw[k] is not None:
                    op.deps[b.lw[k]] = "RAW"
                b.rd[k].append(i)
        for item in writes:
            b, s = item if isinstance(item, tuple) else (item, None)
            for k in _slots(b, s):
                if b.lw[k] is not None and b.lw[k] != i:
                    op.deps.setdefault(b.lw[k], "WAW")
                for r in b.rd[k]:
                    if r != i:
                        op.deps.setdefault(r, "WAR")
                b.lw[k] = i
                b.rd[k] = []
        if dma:
            k = self.dma_rr % N_DMA_SEMS
            self.dma_rr += 1
            op.skey = ("dma", k)
            prev = self.dma_last[k]
            if prev is not None:
                op.deps.setdefault(prev, "SEM")
            self.dma_last[k] = i
        else:
            op.skey = eng
        for p in list(op.deps):
            po = self.ops[p]
            kind = op.deps[p]
            if not po.dma and not dma and po.eng == eng:
                if eng == "pe" or kind == "WAR":
                    del op.deps[p]
                    continue
            po.signal = True
        self.ops.append(op)
        return i

    def finalize(self, sems_alloc):
        counters = {}
        known = {}
        semh = {}

        def sem_for(skey, idx):
            if isinstance(skey, tuple):
                name = ("dma", skey[1])
                val = idx * 16
            else:
                g = (idx - 1) // SEM_CAP
                name = (skey, g)
                val = idx - g * SEM_CAP
            if name not in semh:
                semh[name] = sems_alloc("s_" + "_".join(str(x) for x in name))
            return semh[name], val

        for op in self.ops:
            kn = known.setdefault(op.eng, {})
            for p in sorted(op.deps):
                po = self.ops[p]
                if kn.get(po.skey, 0) >= po.sidx:
                    continue
                op.waits.append(sem_for(po.skey, po.sidx))
                kn[po.skey] = po.sidx
                for k2, v2 in po.snap.items():
                    if kn.get(k2, 0) < v2:
                        kn[k2] = v2
            if op.signal or op.dma:
                c = counters.get(op.skey, 0) + 1
                counters[op.skey] = c
                op.sidx = c
                op.inc = sem_for(op.skey, c)
                op.signal = True
            op.snap = dict(kn)

    def emit(self, engines):
        by_eng = {}
        for op in self.ops:
            by_eng.setdefault(op.eng, []).append(op)

        def make(engname):
            def body(e):
                for op in by_eng.get(engname, []):
                    for (sem, val) in op.waits:
                        e.wait_ge(sem, val)
                    ins = op.fn(e)
                    if op.signal:
                        ins.then_inc(op.inc[0], 16 if op.dma else 1)
            return body

        for engname, deco in engines.items():
            if by_eng.get(engname):
                deco(make(engname))


def build(nt, taps=(), stage=99):
    nc = bass.Bass("TRN2", target_bir_lowering=False)
    S = Sched()
    L = nt * TT
    NBT = 4 * nt

    def din(name, shape, dt=F32):
        return nc.dram_tensor(name, list(shape), dt, kind="ExternalInput").ap()

    xT = din("xT", [1024, L])
    w_in0 = din("w_in0", [1024, 2832])
    w_sm0 = din("w_sm0", [1024, 16])
    w_out0 = din("w_out0", [1024, 1024])
    w_in1 = din("w_in1", [1024, 3088])
    w_out1 = din("w_out1", [1024, 1024])
    w2aug = din("w2aug", [17, 256])
    smask = din("smask", [128, 4, 512])
    w_up = din("w_up", [2, 1024, 4096])
    w_dn = din("w_dn", [2, 4096, 1024])
    nrm = din("nrm", [128, 5, 8])
    rowp = din("rowp", [128, 2048])
    colp = din("colp", [128, 64])
    cmask = din("cmask", [128, 4, 512])
    tri = din("tri", [128, 4, 128])
    outT = nc.dram_tensor("outT", [1024, L], F32, kind="ExternalOutput").ap()
    KH = nc.dram_tensor("KH", [8, 65, L], BF16, kind="Internal").ap()
    wscr = {}
    for nm_, shp_ in (("w_in0", [1024, 2832]), ("w_sm0", [1024, 16]), ("w_out0", [1024, 1024]), ("w_in1", [1024, 3088]),
                      ("w_out1", [1024, 1024]), ("w_up", [2, 1024, 4096]), ("w_dn", [2, 4096, 1024])):
        wscr[nm_] = nc.dram_tensor(nm_ + "_bf", shp_, BF16, kind="Internal").ap()
    VH = nc.dram_tensor("VH", [L, 8, 65], BF16, kind="Internal").ap()
    KH1 = nc.dram_tensor("KH1", [8, 65, L], BF16, kind="Internal").ap()
    VH1 = nc.dram_tensor("VH1", [L, 8, 65], BF16, kind="Internal").ap()
    dbg = {}
    for (tname, shape) in taps:
        dbg[tname] = nc.dram_tensor("dbg_" + tname, list(shape), F32, kind="ExternalOutput").ap()

    es = contextlib.ExitStack()
    with es:
        def sb(name, shape, dt=F32, n=1):
            return Buf(name, es.enter_context(nc.sbuf_tensor(name, list(shape), dt)), n)

        PS = [Buf("ps%d" % i, es.enter_context(nc.psum_tensor("ps%d" % i, [128, 512], F32))) for i in range(8)]
        psc = [0]

        def psum():
            p = PS[psc[0] % 6]
            psc[0] += 1
            return p

        KHb = Buf("KH", KH, 8)
        VHb = Buf("VH", VH, 1)
        KH1b = Buf("KH1", KH1, 8)
        VH1b = Buf("VH1", VH1, 1)
        OUTb = Buf("outT", outT, 1)
        DBGb = Buf("dbg", None, 1)

        def dma(fn, reads=(), writes=(), q="sync"):
            S.add(q, fn, reads, writes, dma=True)

        def pe(fn, reads, writes):
            S.add("pe", fn, reads, writes)

        def act(fn, reads, writes):
            S.add("act", fn, reads, writes)

        def dve(fn, reads, writes):
            S.add("dve", fn, reads, writes)

        def pool(fn, reads, writes):
            S.add("pool", fn, reads, writes)

        hT = sb("hT", [128, 8, TT])
        uT = sb("uT", [128, 8, TT], BF16)
        sq = [sb("sq%d" % i, [128, TT], BF16) for i in range(2)]
        rstd = sb("rstd", [128, TT])
        ones_bf = sb("ones_bf", [128, 128], BF16)
        ones32 = sb("ones32", [128, 128])
        nrm_sb = sb("nrm_sb", [128, 5, 8])
        rowp_sb = sb("rowp_sb", [128, 2048])
        colp_sb = sb("colp_sb", [128, 64])
        cmask_bf = sb("cmask_bf", [128, 4, 512], BF16)
        tri_sb = sb("tri_sb", [128, 4, 128])
        wbf = [sb("wbf%d" % i, [128, 4096], BF16) for i in range(2)]
        wcnt = [0]
        QT = sb("QT", [65, 8, TT], BF16, n=8)
        KTc = sb("KTc", [65, 8, TT], BF16)
        Vc = sb("Vc", [128, 4, 8, 65], BF16)
        sm = sb("sm", [128, 4, 16])
        FH = sb("FH", [128, SEQ // 128, 8])
        Fcar = sb("Fcar", [128, 8])
        fx = sb("fx", [128, 4, 8])
        fsp = sb("fsp", [128, 4, 8])
        frel = sb("frel", [128, 4, 8])
        kbias = sb("kbias", [128, SEQ // 128, 8])
        F8T = sb("F8T", [8, TT], BF16)
        catH = sb("catH", [64, 16, TT], BF16, n=16)
        KTs = [sb("KTs%d" % i, [65, 2048], BF16) for i in range(2)]
        Vs = [sb("Vs%d" % i, [128, 16, 65], BF16) for i in range(2)]
        PT = [sb("PT%d" % i, [128, TT], BF16) for i in range(2)]
        lgt = [sb("lgt%d" % i, [128, TT]) for i in range(2)]
        mlpa = [sb("mlpa0", [128, 4, TT], BF16)] * 2

        dma(lambda e: e.dma_start(out=nrm_sb[:], in_=nrm), writes=[nrm_sb])
        dma(lambda e: e.dma_start(out=rowp_sb[:], in_=rowp), writes=[rowp_sb])
        dma(lambda e: e.dma_start(out=colp_sb[:], in_=colp), writes=[colp_sb])
        dma(lambda e: e.dma_start(out=tri_sb[:], in_=tri), writes=[tri_sb])
        pool(lambda e: e.memset(ones_bf[:], 1.0), [], [ones_bf])
        pool(lambda e: e.memset(ones32[:], 1.0), [], [ones32])
        pool(lambda e: e.memset(KTc[:], 1.0), [], [KTc])
        pool(lambda e: e.memset(Vc[:], 1.0), [], [Vc])
        pool(lambda e: e.memset(Fcar[:], 0.0), [], [Fcar])
        Uincl = tri_sb[:, 0, :]
        ident = tri_sb[:, 1, :]
        FB = rowp_sb[:, 0:8]

        def tap(name, buf, ap):
            if name in dbg:
                dma(lambda e: e.dma_start(out=dbg[name], in_=ap), reads=[buf], writes=[DBGb])

        WSb = Buf("wscr", None, 1)

        def load_w(src_ap, kc, ncols):
            i = wcnt[0] % 2
            wcnt[0] += 1
            bf = wbf[i]
            bfv = bf[:, 0:kc * ncols].rearrange("p (c n) -> p c n", c=kc)
            dma(lambda e: e.dma_start(out=bfv, in_=src_ap.rearrange("(c p) n -> p c n", p=128)), reads=[WSb], writes=[bf], q="pool")
            return bf, bfv

        def rmsnorm(widx, dst, dst_dt_bf16=True):
            p = psum()
            for c in range(8):
                s = sq[c % 2]
                act(lambda e, c=c, s=s: e.activation(out=s[:], in_=hT[:, c, :], func=AF.Square), [hT], [s])
                pe(lambda e, c=c, s=s: e.matmul(p[:], ones_bf[:], s[:], start=(c == 0), stop=(c == 7)), [ones_bf, s], [p])
            act(lambda e: e.activation(out=rstd[:], in_=p[:], func=AF.Sqrt, bias=1e-5, scale=1.0 / 1024), [p], [rstd])
            dve(lambda e: e.reciprocal(out=rstd[:], in_=rstd[:]), [rstd], [rstd])
            for c in range(8):
                dve(lambda e, c=c: e.scalar_tensor_tensor(out=dst[:, c, :], in0=hT[:, c, :], scalar=nrm_sb[:, widx, c:c + 1],
                                                          in1=rstd[:], op0=ALU.mult, op1=ALU.mult), [hT, nrm_sb, rstd], [dst])

        def proj_fm(wb, wv, m0, M, evac):
            p = psum()
            for c in range(8):
                pe(lambda e, c=c: e.matmul(p[0:M, :], wv[:, c, m0:m0 + M], uT[:, c, :], start=(c == 0), stop=(c == 7)), [wb, uT], [p])
            evac(p)

        def proj_tm(wb, wv, n0, N, blk, p, pslice):
            for c in range(8):
                pe(lambda e, c=c: e.matmul(pslice, uT[:, c, blk * 128:(blk + 1) * 128], wv[:, c, n0:n0 + N], start=(c == 0), stop=(c == 7)), [wb, uT], [p])

        def mlp(layer):
            rmsnorm(1 + 2 * layer, uT)
            for g in range(8):
                wb, wv = load_w(w_up[layer, :, g * 512:(g + 1) * 512], 8, 512)
                a = mlpa[g % 2]
                for hc in range(4):
                    def ev(p, hc=hc, a=a):
                        act(lambda e: e.activation(out=lgt[0][:], in_=p[:], func=AF.Copy), [p], [lgt[0]])
                        dve(lambda e: e.scalar_tensor_tensor(out=a[:, hc, :], in0=lgt[0][:], scalar=0.0, in1=lgt[0][:], op0=ALU.max, op1=ALU.mult), [lgt[0]], [a])
                    proj_fm(wb, wv, hc * 128, 128, ev)
                import os
                if os.environ.get('MLPSKIP') == 'down':
                    continue
                for half in range(2):
                    wb2, wv2 = load_w(w_dn[layer, g * 512:(g + 1) * 512, half * 512:(half + 1) * 512], 4, 512)
                    for fc4 in range(4):
                        fc = half * 4 + fc4
                        p = psum()
                        for hc in range(4):
                            pe(lambda e, hc=hc, fc4=fc4, p=p, wv2=wv2, a=a: e.matmul(p[:], wv2[:, hc, fc4 * 128:(fc4 + 1) * 128], a[:, hc, :], start=(hc == 0), stop=(hc == 3)), [wb2, a], [p])
                        dve(lambda e, fc=fc, p=p: e.tensor_tensor(out=hT[:, fc, :], in0=hT[:, fc, :], in1=p[:], op=ALU.add), [hT, p], [hT])

        def out_proj(w_out):
            for colh in range(2):
                ps4 = [psum() for _ in range(4)]
                for ch in range(2):
                    i = wcnt[0] % 2
                    wcnt[0] += 1
                    bf = wbf[i]
                    bfv = bf[0:64, :].rearrange("p (c n) -> p c n", c=8)
                    src = w_out[ch * 512:(ch + 1) * 512, colh * 512:(colh + 1) * 512].rearrange("(c p) n -> p c n", p=64)
                    dma(lambda e, bfv=bfv, src=src: e.dma_start(out=bfv, in_=src), reads=[WSb], writes=[bf], q="pool")
                    for fc in range(4):
                        for c in range(8):
                            cc = ch * 8 + c
                            pe(lambda e, fc=fc, c=c, cc=cc, bfv=bfv, ps4=ps4: e.matmul(ps4[fc][:], bfv[:, c, fc * 128:(fc + 1) * 128], catH[:, cc, :],
                                                                              start=(cc == 0), stop=(cc == 15)), [bf, (catH, cc)], [ps4[fc]])
                for fc in range(4):
                    f = colh * 4 + fc
                    dve(lambda e, f=f, fc=fc, ps4=ps4: e.tensor_tensor(out=hT[:, f, :], in0=hT[:, f, :], in1=ps4[fc][:], op=ALU.add), [hT, ps4[fc]], [hT])


        def MM(out, lhsT, rhs, start, stop, reads, writes):
            S.add("pe", lambda e: e.matmul(out, lhsT, rhs, start=start, stop=stop), reads, writes)

        def TR(out, in_, idn, reads, writes):
            S.add("pe", lambda e: e.transpose(out, in_, idn), reads, writes)

        def ACT(out, in_, func, reads, writes, bias=None, scale=None, accum_out=None):
            kw = {}
            if bias is not None:
                kw["bias"] = bias
            if scale is not None:
                kw["scale"] = scale
            if accum_out is not None:
                kw["accum_out"] = accum_out
            S.add("act", lambda e: e.activation(out=out, in_=in_, func=func, **kw), reads, writes)

        def TT_(out, in0, in1, op, reads, writes, eng="dve"):
            S.add(eng, lambda e: e.tensor_tensor(out=out, in0=in0, in1=in1, op=op), reads, writes)

        def STT(out, in0, scalar, in1, op0, op1, reads, writes, eng="dve"):
            S.add(eng, lambda e: e.scalar_tensor_tensor(out=out, in0=in0, scalar=scalar, in1=in1, op0=op0, op1=op1), reads, writes)

        def TS(out, in0, s1, s2, op0, op1, reads, writes, eng="dve"):
            if s2 is None:
                S.add(eng, lambda e: e.tensor_scalar(out=out, in0=in0, scalar1=s1, scalar2=None, op0=op0), reads, writes)
            else:
                S.add(eng, lambda e: e.tensor_scalar(out=out, in0=in0, scalar1=s1, scalar2=s2, op0=op0, op1=op1), reads, writes)

        def CP(out, in_, reads, writes, eng="dve"):
            S.add(eng, lambda e: e.tensor_copy(out=out, in_=in_), reads, writes)

        def MS(ap, val, writes, eng="pool"):
            S.add(eng, lambda e: e.memset(ap, val), [], writes)

        def RCP(out, in_, reads, writes):
            S.add("dve", lambda e: e.reciprocal(out=out, in_=in_), reads, writes)

        def DMA(out, in_, reads, writes):
            S.add("sync", lambda e: e.dma_start(out=out, in_=in_), reads, writes, dma=True)

        def proj_fm2(wb, wv, m0, M):
            p = psum()
            for c in range(8):
                MM(p[0:M, :], wv[:, c, m0:m0 + M], uT[:, c, :], c == 0, c == 7, [wb, uT], [p])
            return p

        def proj_tm2(wb, wv, n0, N, blk):
            p = psum()
            for c in range(8):
                MM(p[:, 0:N], uT[:, c, blk * 128:(blk + 1) * 128], wv[:, c, n0:n0 + N], c == 0, c == 7, [wb, uT], [p])
            return p

        zs = sb("zs", [128, 4, 512], BF16)
        xpre = sb("xpre", [128, 4, 515])
        bcpre = sb("bcpre", [64, 4, 515])
        halo_x = sb("halo_x", [128, 4, 3])
        halo_bc = sb("halo_bc", [64, 4, 3])
        acc = sb("acc", [128, 512])
        xcv = sb("xcv", [128, 512])
        rden = xcv
        outsb = acc
        x_tok = sb("x_tok", [128, 4, 512], BF16)
        B_tok = sb("B_tok", [128, 4, 128], BF16)
        BTb = sb("BTb", [64, 2, 512], BF16)
        CTb = sb("CTb", [64, 2, 512], BF16)
        dtb = sb("dtb", [128, 4, 8])
        av = sb("av", [128, 4, 8])
        acum = sb("acum", [128, 4, 8])
        Aneg = sb("Aneg", [128, 8])
        Xd = sb("Xd", [128, 8, 128])
        t1 = sb("t1", [128, 8, 128])
        Xe = sb("Xe", [128, 8, 128])
        cdb = sb("cdb", [128, 8])
        Etok = sb("Etok", [128, 8])
        Gs = sb("Gs", [128, 2, 128])
        Wt = sb("Wt", [128, 8, 128], BF16)
        xdt = sb("xdt", [128, 8, 64], BF16)
        Bdec = sb("Bdec", [128, 8, 64], BF16)
        Cdec = sb("Cdec", [64, 8, 128], BF16)
        dte = sb("dte", [128, 8])
        yv = sb("yv", [128, 512])
        yn = sb("yn", [128, 512])
        ssq = sb("ssq", [128, 2])
        Sst = sb("Sst", [64, 8, 64])
        Sbf = sb("Sbf", [64, 8, 64], BF16)
        MS(halo_x[:], 0.0, [halo_x])
        MS(halo_bc[:], 0.0, [halo_bc])
        MS(Sst[:], 0.0, [Sst])
        MS(Sbf[:], 0.0, [Sbf])
        Mneg = tri_sb[:, 2, :]
        DTB = rowp_sb[:, 8:16]
        ALOG = rowp_sb[:, 16:24]
        DSK = rowp_sb[:, 24:536]
        NWS = rowp_sb[:, 536:1048]
        ACT(Aneg[:], ALOG, AF.Exp, [rowp_sb], [Aneg])
        TS(Aneg[:], Aneg[:], -1.0, None, ALU.mult, None, [Aneg], [Aneg])

        import os
        SSDST = int(os.environ.get('SSDST', '99'))
        SSDSUB = int(os.environ.get('SSDSUB', '99'))

        def ssd_tile(ti):
            wb, wv = load_w(w_in0[:, 1544:2056], 8, 512)
            for b in range(4):
                p = proj_tm2(wb, wv, 0, 512, b)
                ACT(zs[:, b, :], p[:], AF.Silu, [p], [zs])
            wb, wv = load_w(w_in0[:, 2056:2568], 8, 512)
            CP(xpre[:, :, 0:3], halo_x[:], [halo_x], [xpre])
            for ch in range(4):
                p = proj_fm2(wb, wv, ch * 128, 128)
                ACT(xpre[:, ch, 3:515], p[:], AF.Copy, [p], [xpre])
            wb, wv = load_w(w_in0[:, 2568:2824], 8, 256)
            CP(bcpre[:, :, 0:3], halo_bc[:], [halo_bc], [bcpre])
            for j in range(4):
                p = proj_fm2(wb, wv, j * 64, 64)
                ACT(bcpre[:, j, 3:515], p[0:64, :], AF.Copy, [p], [bcpre])
            CP(halo_x[:], xpre[:, :, 512:515], [xpre], [halo_x])
            CP(halo_bc[:], bcpre[:, :, 512:515], [bcpre], [halo_bc])
            if SSDST < 2:
                return
            pxs = [psum() for _ in range(4)]
            for ch in range(4):
                TS(acc[:], xpre[:, ch, 0:512], colp_sb[:, ch * 4:ch * 4 + 1], None, ALU.mult, None, [xpre, colp_sb], [acc])
                for i in range(1, 4):
                    STT(acc[:], xpre[:, ch, i:i + 512], colp_sb[:, ch * 4 + i:ch * 4 + i + 1], acc[:], ALU.mult, ALU.add, [xpre, colp_sb, acc], [acc])
                ACT(xcv[:], acc[:], AF.Silu, [acc, colp_sb], [xcv], bias=colp_sb[:, 16 + ch:17 + ch])
                for b in range(4):
                    TR(pxs[b][:, ch * 128:(ch + 1) * 128], xcv[:, b * 128:(b + 1) * 128], ident, [xcv, tri_sb], [pxs[b]])
            for b in range(4):
                ACT(x_tok[:, b, :], pxs[b][:], AF.Copy, [pxs[b]], [x_tok])
            pbt = psum()
            for j in range(4):
                TS(acc[0:64, :], bcpre[:, j, 0:512], colp_sb[0:64, 20 + j * 4:21 + j * 4], None, ALU.mult, None, [bcpre, colp_sb], [acc])
                for i in range(1, 4):
                    STT(acc[0:64, :], bcpre[:, j, i:i + 512], colp_sb[0:64, 20 + j * 4 + i:21 + j * 4 + i], acc[0:64, :], ALU.mult, ALU.add, [bcpre, colp_sb, acc], [acc])
                ACT(xcv[0:64, :], acc[0:64, :], AF.Silu, [acc, colp_sb], [xcv], bias=colp_sb[0:64, 36 + j:37 + j])
                if j < 2:
                    CP(BTb[:, j, :], xcv[0:64, :], [xcv], [BTb])
                    for b in range(4):
                        TR(pbt[:, b * 128 + j * 64:b * 128 + (j + 1) * 64], xcv[0:64, b * 128:(b + 1) * 128], ident[0:64, 0:64], [xcv, tri_sb], [pbt])
                else:
                    CP(CTb[:, j - 2, :], xcv[0:64, :], [xcv], [CTb])
            ACT(B_tok[:].rearrange("p b n -> p (b n)"), pbt[:], AF.Copy, [pbt], [B_tok])
            if SSDST < 3:
                return
            TT_(dtb[:], sm[:, :, 8:16], DTB.unsqueeze(1).to_broadcast([128, 4, 8]), ALU.add, [sm, rowp_sb], [dtb])
            ACT(dtb[:], dtb[:], AF.Exp, [dtb], [dtb])
            ACT(dtb[:], dtb[:], AF.Ln, [dtb], [dtb], bias=1.0)
            TT_(av[:], dtb[:], Aneg[:].unsqueeze(1).to_broadcast([128, 4, 8]), ALU.mult, [dtb, Aneg], [av])
            pa = psum()
            for b in range(4):
                MM(pa[:, b * 8:(b + 1) * 8], Uincl, av[:, b, :], True, True, [tri_sb, av], [pa])
            CP(acum[:].rearrange("p b n -> p (b n)"), pa[:, 0:32], [pa], [acum])
            if SSDST < 4:
                return
            for b in range(4):
                blk = slice(b * 128, (b + 1) * 128)
                for h in range(8):
                    TS(Xd[:, h, :], ident, acum[:, b, h:h + 1], None, ALU.mult, None, [tri_sb, acum], [Xd])
                pab = [psum(), psum()]
                for k in range(2):
                    MM(pab[k][:], ones32[:], Xd[:, 4 * k:4 * k + 4, :].rearrange("p h l -> p (h l)"), True, True, [ones32, Xd], [pab[k]])
                if SSDSUB < 2:
                    continue
                pcl = psum()
                MM(pcl[:, 0:8], ones32[:], av[:, b, :], True, True, [ones32, av], [pcl])
                TT_(dte[:], pcl[:, 0:8], acum[:, b, :], ALU.subtract, [pcl, acum], [dte])
                ACT(dte[:], dte[:], AF.Exp, [dte], [dte])
                ACT(cdb[:], pcl[:, 0:8], AF.Exp, [pcl], [cdb])
                ACT(Etok[:], acum[:, b, :], AF.Exp, [acum], [Etok])
                for h in range(8):
                    TS(Xe[:, h, :], ident, Etok[:, h:h + 1], None, ALU.mult, None, [tri_sb, Etok], [Xe])
                pae = [psum(), psum()]
                for k in range(2):
                    MM(pae[k][:], ones32[:], Xe[:, 4 * k:4 * k + 4, :].rearrange("p h l -> p (h l)"), True, True, [ones32, Xe], [pae[k]])
                if SSDSUB < 3:
                    continue
                for k in range(2):
                    for hh in range(4):
                        h = 4 * k + hh
                        STT(t1[:, h, :], pab[k][:, hh * 128:(hh + 1) * 128], acum[:, b, h:h + 1], Mneg, ALU.subtract, ALU.min, [pab[k], acum, tri_sb], [t1])
                for k in range(2):
                    if True:
                        ACT(t1[:, 4 * k:4 * k + 4, :].rearrange("p h l -> p (h l)"), t1[:, 4 * k:4 * k + 4, :].rearrange("p h l -> p (h l)"), AF.Exp, [t1], [t1])
                if SSDSUB < 4:
                    continue
                pg = psum()
                for g in range(2):
                    MM(pg[:, g * 128:(g + 1) * 128], BTb[:, g, blk], CTb[:, g, blk], True, True, [BTb, CTb], [pg])
                ACT(Gs[:].rearrange("p g l -> p (g l)"), pg[:, 0:256], AF.Copy, [pg], [Gs])
                if SSDSUB < 5:
                    continue
                for g in range(2):
                    TT_(Wt[:, 4 * g:4 * g + 4, :], t1[:, 4 * g:4 * g + 4, :], Gs[:, g, :].unsqueeze(1).to_broadcast([128, 4, 128]), ALU.mult, [t1, Gs], [Wt])
                    TT_(Cdec[:, 4 * g:4 * g + 4, :], pae[g][0:64, :].rearrange("p (h l) -> p h l", h=4), CTb[:, g, blk].unsqueeze(1).to_broadcast([64, 4, 128]), ALU.mult, [pae[g], CTb], [Cdec])
                if SSDSUB < 6:
                    continue
                for h in range(8):
                    g = h // 4
                    TS(xdt[:, h, :], x_tok[:, b, h * 64:(h + 1) * 64], dtb[:, b, h:h + 1], None, ALU.mult, None, [x_tok, dtb], [xdt])
                    TS(Bdec[:, h, :], B_tok[:, b, g * 64:(g + 1) * 64], dte[:, h:h + 1], None, ALU.mult, None, [B_tok, dte], [Bdec])
                if SSDST < 5:
                    continue
                py = psum()
                for h in range(8):
                    MM(py[:, h * 64:(h + 1) * 64], Wt[:, h, :], xdt[:, h, :], True, False, [Wt, xdt], [py])
                    MM(py[:, h * 64:(h + 1) * 64], Cdec[:, h, :], Sbf[:, h, :], False, True, [Cdec, Sbf], [py])
                TT_(yv[:], x_tok[:, b, :], DSK, ALU.mult, [x_tok, rowp_sb], [yv])
                TT_(yv[:], yv[:], py[:], ALU.add, [yv, py], [yv])
                TT_(yv[:], yv[:], zs[:, b, :], ALU.mult, [yv, zs], [yv])
                MS(ssq[:], 0.0, [ssq], eng="dve")
                for g in range(2):
                    ACT(yn[:, g * 256:(g + 1) * 256], yv[:, g * 256:(g + 1) * 256], AF.Square, [yv], [yn, ssq], accum_out=ssq[:, g:g + 1])
                ACT(ssq[:], ssq[:], AF.Sqrt, [ssq], [ssq], bias=1e-5, scale=1.0 / 256)
                RCP(ssq[:], ssq[:], [ssq], [ssq])
                for g in range(2):
                    STT(yn[:, g * 256:(g + 1) * 256], yv[:, g * 256:(g + 1) * 256], ssq[:, g:g + 1], NWS[:, g * 256:(g + 1) * 256], ALU.mult, ALU.mult, [yv, ssq, rowp_sb], [yn])
                pT = [psum(), psum()]
                for h in range(8):
                    TR(pT[h // 4][0:64, (h % 4) * 128:(h % 4 + 1) * 128], yn[:, h * 64:(h + 1) * 64], ident, [yn, tri_sb], [pT[h // 4]])
                for k in range(2):
                    ACT(catH[:, 8 + 4 * k:12 + 4 * k, blk], pT[k][0:64, :].rearrange("p (h l) -> p h l", h=4), AF.Copy, [pT[k]], [(catH, s_) for s_ in range(8 + 4 * k, 12 + 4 * k)])
                if SSDST < 6:
                    continue
                pl = psum()
                for h in range(8):
                    MM(pl[0:64, h * 64:(h + 1) * 64], Bdec[:, h, :], xdt[:, h, :], True, True, [Bdec, xdt], [pl])
                for h in range(8):
                    STT(Sst[:, h, :], Sst[:, h, :], cdb[0:64, h:h + 1], pl[0:64, h * 64:(h + 1) * 64], ALU.mult, ALU.add, [Sst, cdb, pl], [Sst])
                ACT(Sbf[:], Sst[:], AF.Copy, [Sst], [Sbf])


        smask_bf = sb("smask_bf", [128, 4, 512], BF16)
        tri_bf = sb("tri_bf", [128, 2, 128], BF16)
        SG = sb("SG", [64, 4, 128])
        SGbf = sb("SGbf", [64, 4, 128], BF16)
        glT = sb("glT", [17, 512], BF16)
        W2b = sb("W2b", [17, 256], BF16)
        ssq4 = sb("ssq4", [128, 4])
        spb = [sb("spb%d" % i_, [128, TT]) for i_ in range(2)]
        hl = [sb("hl%d" % i_, [128, 2, TT], BF16) for i_ in range(2)]
        tlb = [sb("tlb%d" % i_, [128, TT]) for i_ in range(2)]
        for j_ in range(4):
            DMA(lgt[0][:], smask[:, j_, :], [], [lgt[0]])
            CP(smask_bf[:, j_, :], lgt[0][:], [lgt[0]], [smask_bf], eng="pool")
            DMA(lgt[1][:], cmask[:, j_, :], [], [lgt[1]])
            CP(cmask_bf[:, j_, :], lgt[1][:], [lgt[1]], [cmask_bf], eng="pool")
        CP(tri_bf[:, 0, :], tri_sb[:, 3, :], [tri_sb], [tri_bf], eng="pool")
        CP(tri_bf[:, 1, :], tri_sb[:, 1, :], [tri_sb], [tri_bf], eng="pool")
        DMA(lgt[1][0:17, 0:256], w2aug, [], [lgt[1]])
        CP(W2b[:], lgt[1][0:17, 0:256], [lgt[1]], [W2b], eng="pool")
        MS(SG[:], 0.0, [SG])
        MS(SGbf[:], 0.0, [SGbf])
        MS(glT[:], 1.0, [glT])
        Usuf_bf = tri_bf[:, 0, :]
        ident_bf = tri_bf[:, 1, :]
        GN = rowp_sb[:, 1048:1560]
        Rb = yv
        lrf = yn
        hi_t = mlpa[0]
        GQT = xpre
        GKT = bcpre
        gv = x_tok
        grs = zs
        gk_tok = Wt
        la = Xd
        gtok = Gs
        kdec = xdt
        tmpg = t1
        egb = Xe
        qkd = Cdec
        ATb = Bdec
        o_sb = yv
        on_ = yn
        GNR = acc

        def sb_tile(ti):
            t0 = ti * TT
            nkb = 4 * (ti + 1)
            wb, wv = load_w(w_in1[:, 0:512], 8, 512)
            for h in range(8):
                p = proj_fm2(wb, wv, h * 64, 64)
                ACT(QT[0:64, h, :], p[0:64, :], AF.Copy, [p], [(QT, h)], scale=0.125)
            wb, wv = load_w(w_in1[:, 512:1024], 8, 512)
            for h in range(8):
                p = proj_fm2(wb, wv, h * 64, 64)
                ACT(KTc[0:64, h, :], p[0:64, :], AF.Copy, [p], [KTc])
            for h in range(8):
                DMA(KH1[h, :, t0:t0 + TT], KTc[:, h, :], [KTc], [(KH1b, h)])
            wb, wv = load_w(w_in1[:, 1024:1536], 8, 512)
            for b in range(4):
                p = proj_tm2(wb, wv, 0, 512, b)
                ACT(Vc[:, b, :, 0:64], p[:].rearrange("p (h d) -> p h d", h=8), AF.Copy, [p], [Vc])
            DMA(VH1[t0:t0 + TT].rearrange("(b p) h d -> p b h d", p=128), Vc[:], [Vc], [VH1b])
            p_o = PS[6]
            for h in range(8):
                MS(Rb[:], 0.0, [Rb], eng="dve")
                nch = (nkb + 15) // 16
                blocks = []
                for kc in reversed(range(nch)):
                    nb = min(16, nkb - kc * 16)
                    for j in reversed(range(nb)):
                        blocks.append((kc, j, nb))
                st = {}
                loaded = {}

                def A1(i):
                    kc, j, nb = blocks[i]
                    if kc not in loaded:
                        k0 = kc * 2048
                        kt = KTs[(h + kc) % 2]
                        vs = Vs[(h + kc) % 2]
                        DMA(kt[:, 0:nb * 128], KH1[h, :, k0:k0 + nb * 128], [(KH1b, h)], [kt])
                        DMA(vs[:, 0:nb, :], VH1[k0:k0 + nb * 128, h, :].rearrange("(b p) d -> p b d", p=128), [VH1b], [vs])
                        loaded[kc] = (kt, vs)
                    kt, vs = loaded[kc]
                    kb = kc * 16 + j
                    dj = kb - 4 * ti
                    ksl = kt[0:64, j * 128:(j + 1) * 128]
                    pz = psum()
                    MM(pz[:], ksl, QT[0:64, h, :], True, dj < 0, [kt, (QT, h)], [pz])
                    if dj >= 0:
                        MM(pz[:], ident_bf, smask_bf[:, dj, :], False, True, [tri_bf, smask_bf], [pz])
                    st[i] = dict(kt=kt, vs=vs, kb=kb, dj=dj, ksl=ksl, pz=pz, par=kb % 2, j=j)

                def A2act(i):
                    d = st[i]
                    par = d["par"]
                    eb_, sp_, hl_ = lgt[par], spb[par], hl[par]
                    ACT(eb_[:], d["pz"][:], AF.Exp, [d["pz"]], [eb_])
                    ACT(sp_[:], eb_[:], AF.Ln, [eb_], [sp_], bias=1.0)
                    if par == 0:
                        ACT(hl_[:, 0, :], sp_[:], AF.Copy, [sp_], [hl_], scale=-1.0)
                    else:
                        TS(hl_[:, 0, :], sp_[:], -1.0, None, ALU.mult, None, [sp_], [hl_], eng="dve")

                def A2dve(i):
                    d = st[i]
                    par = d["par"]
                    sp_, hl_ = spb[par], hl[par]
                    STT(hl_[:, 1, :], sp_[:], -1.0, hl_[:, 0, :], ALU.mult, ALU.subtract, [sp_, hl_], [hl_])

                def B_(i):
                    d = st[i]
                    hl_ = hl[d["par"]]
                    pL = psum()
                    MM(pL[:], d["ksl"], QT[0:64, h, :], True, False, [d["kt"], (QT, h)], [pL])
                    if d["dj"] >= 0:
                        MM(pL[:], ident_bf, smask_bf[:, d["dj"], :], False, False, [tri_bf, smask_bf], [pL])
                    MM(pL[:], Usuf_bf, hl_[:, 0, :], False, False, [tri_bf, hl_], [pL])
                    MM(pL[:], Usuf_bf, hl_[:, 1, :], False, True, [tri_bf, hl_], [pL])
                    pR = psum()
                    MM(pR[:], ones_bf[:], hl_[:, 0, :], True, False, [ones_bf, hl_], [pR])
                    MM(pR[:], ones_bf[:], hl_[:, 1, :], False, True, [ones_bf, hl_], [pR])
                    d["pL"] = pL
                    d["pR"] = pR

                def C1a(i):
                    d = st[i]
                    tl_ = tlb[d["par"]]
                    TT_(tl_[:], d["pL"][:], Rb[:], ALU.add, [d["pL"], Rb], [tl_])
                    TT_(Rb[:], Rb[:], d["pR"][:], ALU.add, [Rb, d["pR"]], [Rb])

                def C1b(i):
                    d = st[i]
                    tl_ = tlb[d["par"]]
                    ACT(PT[d["par"]][:], tl_[:], AF.Exp, [tl_], [PT[d["par"]]])

                def C2(i):
                    d = st[i]
                    MM(p_o[0:64, :], d["vs"][:, d["j"], 0:64], PT[d["par"]][:], d["kb"] == nkb - 1, d["kb"] == 0, [d["vs"], PT[d["par"]]], [p_o])
                    del st[i]

                nbk = len(blocks)
                A1(0)
                for i in range(nbk + 2):
                    if 0 <= i - 2 < nbk:
                        C1a(i - 2)
                    if i < nbk:
                        A2act(i)
                    if 0 <= i - 2 < nbk:
                        C1b(i - 2)
                    if i < nbk:
                        A2dve(i)
                    if 0 <= i - 1 < nbk:
                        B_(i - 1)
                    if i + 1 < nbk:
                        A1(i + 1)
                    if 0 <= i - 2 < nbk:
                        C2(i - 2)
                ACT(catH[:, h, :], p_o[0:64, :], AF.Copy, [p_o], [(catH, h)])

        def gla_tile(ti):
            wb, wv = load_w(w_in1[:, 1536:2048], 8, 512)
            for h in range(4):
                p = proj_fm2(wb, wv, h * 64, 64)
                ACT(GQT[0:64, h, 0:512], p[0:64, :], AF.Copy, [p], [GQT])
            for h in range(4):
                p = proj_fm2(wb, wv, 256 + h * 64, 64)
                ACT(GKT[0:64, h, 0:512], p[0:64, :], AF.Copy, [p], [GKT])
            gkv = gk_tok[:].rearrange("p (b x) l -> p b (x l)", b=4)
            for b in range(4):
                p = proj_tm2(wb, wv, 256, 256, b)
                ACT(gkv[:, b, :], p[:, 0:256], AF.Copy, [p], [gk_tok])
            wb, wv = load_w(w_in1[:, 2048:2560], 8, 512)
            for b in range(4):
                p = proj_tm2(wb, wv, 0, 512, b)
                ACT(gv[:, b, :], p[:], AF.Copy, [p], [gv])
            wb, wv = load_w(w_in1[:, 2576:3088], 8, 512)
            for b in range(4):
                p = proj_tm2(wb, wv, 0, 512, b)
                ACT(grs[:, b, :], p[:], AF.Silu, [p], [grs])
            wb, wv = load_w(w_in1[:, 2560:2576], 8, 16)
            p = proj_fm2(wb, wv, 0, 16)
            ACT(glT[0:16, :], p[0:16, :], AF.Copy, [p], [glT])
            lav = la[:].rearrange("p (b x) l -> p b (x l)", b=4)
            for b in range(4):
                blk = slice(b * 128, (b + 1) * 128)
                pgl = psum()
                MM(pgl[:, 0:256], glT[0:17, blk], W2b[0:17, :], True, True, [glT, W2b], [pgl])
                ACT(lav[:, b, :], pgl[:, 0:256], AF.Exp, [pgl], [la], scale=-1.0)
            for b in range(4):
                ACT(lav[:, b, :], lav[:, b, :], AF.Ln, [la], [la], bias=1.0)
                TS(lav[:, b, :], lav[:, b, :], -1.0 / 16.0, None, ALU.mult, None, [la], [la])
            gtv = gtok[:].rearrange("p g l -> p (g l)")
            tmv = tmpg[:].rearrange("p h l -> p (h l)")
            kdv = kdec[:].rearrange("p h d -> p (h d)")
            for b in range(4):
                blk = slice(b * 128, (b + 1) * 128)
                pgt = psum()
                MM(pgt[:, 0:256], Uincl, lav[:, b, :], True, True, [tri_sb, la], [pgt])
                pgs = psum()
                MM(pgs[:, 0:256], ones32[:], lav[:, b, :], True, True, [ones32, la], [pgs])
                CP(gtv, pgt[:, 0:256], [pgt], [gtok])
                TT_(tmv[:, 0:256], pgs[:, 0:256], gtv, ALU.subtract, [pgs, gtok], [tmpg])
                ACT(tmv[:, 0:256], tmv[:, 0:256], AF.Exp, [tmpg], [tmpg])
                TT_(kdv[:, 0:256], gkv[:, b, :], tmv[:, 0:256], ALU.mult, [gk_tok, tmpg], [kdec])
                pgT = psum()
                for h in range(4):
                    MM(pgT[0:64, h * 128:(h + 1) * 128], lav[:, b, h * 64:(h + 1) * 64], Uincl, True, True, [la, tri_sb], [pgT])
                CP(tmv[0:64, 512:1024], pgT[0:64, :], [pgT], [tmpg])
                egv = egb[0:64, 0:4, :].rearrange("p h l -> p (h l)")
                engv = egb[0:64, 4:8, :].rearrange("p h l -> p (h l)")
                ACT(egv, tmv[0:64, 512:1024], AF.Exp, [tmpg], [egb])
                ACT(engv, tmv[0:64, 512:1024], AF.Exp, [tmpg], [egb], scale=-1.0)
                STT(qkd[:, 0:4, :], GQT[0:64, :, blk], 0.125, egb[0:64, 0:4, :], ALU.mult, ALU.mult, [GQT, egb], [qkd])
                TT_(qkd[:, 4:8, :], GKT[0:64, :, blk], egb[0:64, 4:8, :], ALU.mult, [GKT, egb], [qkd])
                pA = psum()
                for h in range(4):
                    MM(pA[:, h * 128:(h + 1) * 128], qkd[:, 4 + h, :], qkd[:, h, :], True, True, [qkd], [pA])
                atv = ATb[:].rearrange("p (h x) d -> p h (x d)", h=4)
                TT_(atv, pA[:].rearrange("p (h l) -> p h l", h=4), Uincl.unsqueeze(1).to_broadcast([128, 4, 128]), ALU.mult, [pA, tri_sb], [ATb])
                po = psum()
                for h in range(4):
                    MM(po[:, h * 128:(h + 1) * 128], atv[:, h, :], gv[:, b, h * 128:(h + 1) * 128], True, False, [ATb, gv], [po])
                    MM(po[:, h * 128:(h + 1) * 128], qkd[:, h, :], SGbf[:, h, :], False, True, [qkd, SGbf], [po])
                CP(o_sb[:], po[:], [po], [o_sb])
                MS(ssq4[:], 0.0, [ssq4], eng="dve")
                for h in range(4):
                    ACT(on_[:, h * 128:(h + 1) * 128], o_sb[:, h * 128:(h + 1) * 128], AF.Square, [o_sb], [on_, ssq4], accum_out=ssq4[:, h:h + 1])
                ACT(ssq4[:], ssq4[:], AF.Sqrt, [ssq4], [ssq4], bias=1e-5, scale=1.0 / 128)
                RCP(ssq4[:], ssq4[:], [ssq4], [ssq4])
                TT_(GNR[:], grs[:, b, :], GN, ALU.mult, [grs, rowp_sb], [GNR])
                for h in range(4):
                    STT(on_[:, h * 128:(h + 1) * 128], o_sb[:, h * 128:(h + 1) * 128], ssq4[:, h:h + 1], GNR[:, h * 128:(h + 1) * 128], ALU.mult, ALU.mult, [o_sb, ssq4, GNR], [on_])
                pT = [psum(), psum()]
                for c in range(8):
                    TR(pT[c // 4][0:64, (c % 4) * 128:(c % 4 + 1) * 128], on_[:, c * 64:(c + 1) * 64], ident, [on_, tri_sb], [pT[c // 4]])
                for k in range(2):
                    ACT(catH[:, 8 + 4 * k:12 + 4 * k, blk], pT[k][0:64, :].rearrange("p (h l) -> p h l", h=4), AF.Copy, [pT[k]], [(catH, s_) for s_ in range(8 + 4 * k, 12 + 4 * k)])
                pl = psum()
                for h in range(4):
                    MM(pl[0:64, h * 128:(h + 1) * 128], kdv[:, h * 64:(h + 1) * 64], gv[:, b, h * 128:(h + 1) * 128], True, True, [kdec, gv], [pl])
                for h in range(4):
                    STT(SG[:, h, :], SG[:, h, :], egb[0:64, h, 127:128], pl[0:64, h * 128:(h + 1) * 128], ALU.mult, ALU.add, [SG, egb, pl], [SG])
                ACT(SGbf[:], SG[:], AF.Copy, [SG], [SGbf])

        hTf = hT[:].rearrange("p c t -> p (c t)")
        pieces = []
        for nm_, src_ in (("w_sm0", w_sm0), ("w_in0", w_in0), ("w_out0", w_out0), ("w_in1", w_in1), ("w_out1", w_out1)):
            R_, C_ = src_.shape
            for r0 in range(0, R_, 128):
                pieces.append((src_[r0:r0 + 128, :], wscr[nm_][r0:r0 + 128, :], C_))
        for l_ in range(2):
            for r0 in range(0, 1024, 128):
                pieces.append((w_up[l_, r0:r0 + 128, :], wscr["w_up"][l_, r0:r0 + 128, :], 4096))
            for r0 in range(0, 4096, 512):
                pieces.append((w_dn[l_, r0:r0 + 512, :].rearrange("(a p) n -> p a n", p=128), wscr["w_dn"][l_, r0:r0 + 512, :].rearrange("(a p) n -> p a n", p=128), (4, 1024)))
        for pi_, (src_, dst_, C_) in enumerate(pieces):
            bfb = wbf[pi_ % 2]
            if isinstance(C_, tuple):
                a_, n_ = C_
                stv_ = hTf[:, 0:a_ * n_].rearrange("p (a n) -> p a n", a=a_)
                bfv_ = bfb[:, 0:a_ * n_].rearrange("p (a n) -> p a n", a=a_)
                for a1 in range(a_):
                    for hf in range(2):
                        DMA(stv_[:, a1, hf * 512:(hf + 1) * 512], src_[:, a1, hf * 512:(hf + 1) * 512], [], [hT])
                tot_ = a_ * n_
            else:
                stv_ = hTf[:, 0:C_]
                bfv_ = bfb[:, 0:C_]
                for c0 in range(0, C_, 512):
                    c1 = min(C_, c0 + 512)
                    DMA(stv_[:, c0:c1], src_[:, c0:c1], [], [hT])
                tot_ = C_
            half_ = (tot_ + 1) // 2
            CP(bfb[:, 0:half_], hTf[:, 0:half_], [hT], [bfb], eng="dve")
            if tot_ > half_:
                ACT(bfb[:, half_:tot_], hTf[:, half_:tot_], AF.Copy, [hT], [bfb])
            if isinstance(C_, tuple):
                for a1 in range(a_):
                    DMA(dst_[:, a1, :], bfv_[:, a1, :], [bfb], [WSb])
            else:
                DMA(dst_, bfv_, [bfb], [WSb])
        w_sm0 = wscr["w_sm0"]
        w_in0 = wscr["w_in0"]
        w_out0 = wscr["w_out0"]
        w_in1 = wscr["w_in1"]
        w_out1 = wscr["w_out1"]
        w_up = wscr["w_up"]
        w_dn = wscr["w_dn"]

        for ti in range(nt):
            t0 = ti * TT
            dma(lambda e, t0=t0: e.dma_start(out=hT[:], in_=xT[:, t0:t0 + TT].rearrange("(c p) t -> p c t", p=128)), writes=[hT])
            rmsnorm(0, uT)
            wb, wv = load_w(w_sm0, 8, 16)
            p = psum()
            for b in range(4):
                proj_tm(wb, wv, 0, 16, b, p, p[:, b * 16:(b + 1) * 16])
            act(lambda e, p=p: e.activation(out=sm[:].rearrange("p b n -> p (b n)"), in_=p[:, 0:64], func=AF.Copy), [p], [sm])
            dve(lambda e: e.tensor_tensor(out=fx[:], in0=sm[:, :, 0:8], in1=FB.unsqueeze(1).to_broadcast([128, 4, 8]), op=ALU.add), [sm, rowp_sb], [fx])
            act(lambda e: e.activation(out=fx[:], in_=fx[:], func=AF.Exp, scale=-1.0), [fx], [fx])
            act(lambda e: e.activation(out=fsp[:], in_=fx[:], func=AF.Ln, bias=1.0), [fx], [fsp])
            pc = psum()
            pcs = psum()
            for b in range(4):
                pe(lambda e, b=b, pc=pc, pcs=pcs: e.matmul(pc[:, b * 8:(b + 1) * 8], Uincl, fsp[:, b, :], start=True, stop=True), [tri_sb, fsp], [pc])
                pe(lambda e, b=b, pc=pc, pcs=pcs: e.matmul(pcs[:, b * 8:(b + 1) * 8], ones32[:], fsp[:, b, :], start=True, stop=True), [ones32, fsp], [pcs])
            for b in range(4):
                if b == 0:
                    dve(lambda e, pc=pc, pcs=pcs: e.tensor_copy(out=frel[:, 0, :], in_=pc[:, 0:8]), [pc], [frel])
                else:
                    dve(lambda e, b=b, pc=pc, pcs=pcs: e.tensor_tensor(out=frel[:, b, :], in0=pc[:, b * 8:(b + 1) * 8], in1=fx[:, b, :], op=ALU.add), [pc, fx], [frel])
                if b < 3:
                    if b == 0:
                        dve(lambda e, pc=pc, pcs=pcs: e.tensor_copy(out=fx[:, 1, :], in_=pcs[:, 0:8]), [pcs], [fx])
                    else:
                        dve(lambda e, b=b, pc=pc, pcs=pcs: e.tensor_tensor(out=fx[:, b + 1, :], in0=fx[:, b, :], in1=pcs[:, b * 8:(b + 1) * 8], op=ALU.add), [pcs, fx], [fx])
            dve(lambda e, ti=ti: e.tensor_tensor(out=FH[:, ti * 4:(ti + 1) * 4, :], in0=frel[:], in1=Fcar[:].unsqueeze(1).to_broadcast([128, 4, 8]), op=ALU.add),
                [frel, Fcar], [FH])
            nkb = 4 * (ti + 1)
            dve(lambda e, nkb=nkb: e.tensor_tensor(out=kbias[:, 0:nkb, :], in0=FH[:, 0:nkb, :], in1=Fcar[:].unsqueeze(1).to_broadcast([128, nkb, 8]), op=ALU.subtract),
                [FH, Fcar], [kbias])
            pt = psum()
            for b in range(4):
                pe(lambda e, b=b, pt=pt: e.transpose(pt[0:8, b * 128:(b + 1) * 128], frel[:, b, :], ident), [frel, tri_sb], [pt])
            act(lambda e, pt=pt: e.activation(out=F8T[:], in_=pt[0:8, :], func=AF.Copy, scale=-8.0), [pt], [F8T])
            dve(lambda e, pc=pc, pcs=pcs: e.tensor_tensor(out=fx[:, 0, :], in0=fx[:, 3, :], in1=pcs[:, 24:32], op=ALU.add), [fx, pcs], [fx])
            dve(lambda e: e.tensor_tensor(out=Fcar[:], in0=Fcar[:], in1=fx[:, 0, :], op=ALU.add), [fx, Fcar], [Fcar])
            if stage < 2:
                break
            if stage == 77:
                mlp(0)
                break
            wb, wv = load_w(w_in0[:, 0:512], 8, 512)
            for h in range(8):
                proj_fm(wb, wv, h * 64, 64, lambda p, h=h: act(lambda e: e.activation(out=QT[0:64, h, :], in_=p[0:64, :], func=AF.Copy), [p], [(QT, h)]))
            for h in range(8):
                dma(lambda e, h=h: e.dma_start(out=QT[64:65, h, :], in_=F8T[h:h + 1, :]), reads=[F8T], writes=[(QT, h)])
            wb, wv = load_w(w_in0[:, 512:1024], 8, 512)
            for h in range(8):
                proj_fm(wb, wv, h * 64, 64, lambda p, h=h: act(lambda e: e.activation(out=KTc[0:64, h, :], in_=p[0:64, :], func=AF.Copy), [p], [KTc]))
            for h in range(8):
                dma(lambda e, h=h, t0=t0: e.dma_start(out=KH[h, :, t0:t0 + TT], in_=KTc[:, h, :]), reads=[KTc], writes=[(KHb, h)])
            wb, wv = load_w(w_in0[:, 1024:1536], 8, 512)
            for b in range(4):
                p = psum()
                proj_tm(wb, wv, 0, 512, b, p, p[:])
                act(lambda e, b=b, p=p: e.activation(out=Vc[:, b, :, 0:64], in_=p[:].rearrange("p (h d) -> p h d", h=8), func=AF.Copy), [p], [Vc])
            dma(lambda e, t0=t0: e.dma_start(out=VH[t0:t0 + TT].rearrange("(b p) h d -> p b h d", p=128), in_=Vc[:]), reads=[Vc], writes=[VHb])
            if stage < 3:
                break
            for h in range(8):
                p_o = PS[6]
                p_d = PS[7]
                pend = None
                for kc in range((nkb + 15) // 16):
                    k0 = kc * 2048
                    nb = min(16, nkb - kc * 16)
                    kt = KTs[(h + kc) % 2]
                    vs = Vs[(h + kc) % 2]
                    DMA(kt[:, 0:nb * 128], KH[h, :, k0:k0 + nb * 128], [(KHb, h)], [kt])
                    DMA(vs[:, 0:nb, :], VH[k0:k0 + nb * 128, h, :].rearrange("(b p) d -> p b d", p=128), [VHb], [vs])
                    for j in range(nb):
                        kb = kc * 16 + j
                        dj = kb - 4 * ti
                        ps_ = psum()
                        MM(ps_[:], kt[:, j * 128:(j + 1) * 128], QT[:, h, :], True, dj < 0, [kt, (QT, h)], [ps_])
                        if dj >= 0:
                            MM(ps_[:], tri_bf[:, 1, :], cmask_bf[:, dj, :], False, True, [tri_bf, cmask_bf], [ps_])
                        ptile = PT[kb % 2]
                        ACT(ptile[:], ps_[:], AF.Exp, [ps_, kbias], [ptile], bias=kbias[:, kb, h:h + 1], scale=0.125)

                        def st2(vs=vs, j=j, ptile=ptile, kb=kb):
                            MM(p_o[0:64, :], vs[:, j, 0:64], ptile[:], kb == 0, kb == nkb - 1, [vs, ptile], [p_o])
                            MM(p_d[0:64, :], ones_bf[:, 0:64], ptile[:], kb == 0, kb == nkb - 1, [ones_bf, ptile], [p_d])
                        if pend is not None:
                            pend()
                        pend = st2
                pend()
                dve(lambda e, p_d=p_d: e.reciprocal(out=rden[0:64, :], in_=p_d[0:64, :]), [p_d], [rden])
                dve(lambda e, h=h, p_o=p_o: e.tensor_tensor(out=catH[:, h, :], in0=p_o[0:64, :], in1=rden[0:64, :], op=ALU.mult), [p_o, rden], [(catH, h)])
            if stage < 4:
                break
            ssd_tile(ti)
            out_proj(w_out0)
            if stage >= 5:
                mlp(0)
            if stage >= 6:
                rmsnorm(2, uT)
                sb_tile(ti)
                if stage >= 7:
                    gla_tile(ti)
                else:
                    for h_ in range(8, 16):
                        MS(catH[:, h_, :], 0.0, [(catH, h_)])
                out_proj(w_out1)
                if stage >= 8:
                    mlp(1)
            p = psum()
            for c in range(8):
                s = sq[c % 2]
                act(lambda e, c=c, s=s: e.activation(out=s[:], in_=hT[:, c, :], func=AF.Square), [hT], [s])
                pe(lambda e, c=c, s=s, p=p: e.matmul(p[:], ones_bf[:], s[:], start=(c == 0), stop=(c == 7)), [ones_bf, s], [p])
            act(lambda e, p=p: e.activation(out=rstd[:], in_=p[:], func=AF.Sqrt, bias=1e-5, scale=1.0 / 1024), [p], [rstd])
            dve(lambda e: e.reciprocal(out=rstd[:], in_=rstd[:]), [rstd], [rstd])
            for c in range(8):
                dve(lambda e, c=c: e.scalar_tensor_tensor(out=outsb[:], in0=hT[:, c, :], scalar=nrm_sb[:, 4, c:c + 1], in1=rstd[:], op0=ALU.mult, op1=ALU.mult),
                    [hT, nrm_sb, rstd], [outsb])
                dma(lambda e, c=c, t0=t0: e.dma_start(out=outT[c * 128:(c + 1) * 128, t0:t0 + TT], in_=outsb[:]), reads=[outsb], writes=[OUTb])
        S.add("sync", lambda e: e.nop(), reads=[OUTb, DBGb])
        S.finalize(lambda name: es.enter_context(nc.semaphore(name)))
        block = es.enter_context(nc.Block())
        S.emit({"sync": block.sync, "act": block.scalar, "dve": block.vector, "pool": block.gpsimd, "pe": block.tensor})
    return nc


def prep_inputs(inp, b, nt):
    L = nt * TT
    f = np.float32
    d = {}
    d["xT"] = np.ascontiguousarray(inp["x"][b, :L, :].T).astype(f)
    w0 = np.asarray(inp["w_in_ab"][0], f)
    d["w_in0"] = np.ascontiguousarray(w0)
    d["w_sm0"] = np.ascontiguousarray(np.concatenate([w0[:, 1536:1544], w0[:, 2824:2832]], axis=1))
    d["w_out0"] = np.ascontiguousarray(inp["w_out_ab"][0], f)
    d["w_in1"] = np.ascontiguousarray(inp["w_in_cd"][0], f)
    d["w_out1"] = np.ascontiguousarray(inp["w_out_cd"][0], f)
    d["w2aug"] = np.ascontiguousarray(np.concatenate([inp["gla_gate_w2"][0], inp["gla_gate_b"][0][None, :]], 0), f)
    d["w_up"] = np.ascontiguousarray(inp["w_mlp_up"], f)
    d["w_dn"] = np.ascontiguousarray(inp["w_mlp_down"], f)
    nr = np.stack([inp["norm_mix"][0], inp["norm_mlp"][0], inp["norm_mix"][1], inp["norm_mlp"][1], inp["norm_final"]], 0).astype(f)
    d["nrm"] = np.ascontiguousarray(nr.reshape(5, 8, 128).transpose(2, 0, 1))
    rowp = np.zeros((2048,), f)
    rowp[0:8] = inp["fox_f_bias"][0]
    rowp[8:16] = inp["ssd_dt_bias"][0]
    rowp[16:24] = inp["ssd_a_log"][0]
    rowp[24:536] = np.repeat(inp["ssd_d"][0], 64)
    rowp[536:1048] = inp["ssd_norm"][0]
    rowp[1048:1560] = np.tile(inp["gla_norm"][0], 4)
    d["rowp"] = np.ascontiguousarray(np.broadcast_to(rowp[None, :], (128, 2048)))
    colp = np.zeros((128, 64), f)
    cw = np.asarray(inp["ssd_conv_w"][0], f)
    cb = np.asarray(inp["ssd_conv_b"][0], f)
    for ch in range(4):
        colp[:, ch * 4:ch * 4 + 4] = cw[:, ch * 128:(ch + 1) * 128].T
        colp[:, 16 + ch] = cb[ch * 128:(ch + 1) * 128]
    for j in range(4):
        colp[0:64, 20 + j * 4:24 + j * 4] = cw[:, 512 + j * 64:512 + (j + 1) * 64].T
        colp[0:64, 36 + j] = cb[512 + j * 64:512 + (j + 1) * 64]
    d["colp"] = colp
    cm = np.zeros((128, 4, 512), f)
    pp = np.arange(128)[:, None]
    ii = np.arange(512)[None, :]
    for j in range(4):
        cm[:, j, :] = np.where(pp + 128 * j <= ii, 0.0, NEG)
    d["cmask"] = cm
    tri = np.zeros((128, 4, 128), f)
    tri[:, 0, :] = (pp <= np.arange(128)[None, :])
    tri[:, 1, :] = np.eye(128)
    tri[:, 2, :] = np.where(np.arange(128)[None, :] >= pp, 0.0, NEG)
    tri[:, 3, :] = (pp >= np.arange(128)[None, :])
    d["tri"] = tri
    sm_ = np.zeros((128, 4, 512), f)
    for j in range(4):
        sm_[:, j, :] = np.where(pp + 128 * j < ii, 0.0, NEG)
    d["smask"] = sm_
    return d


_CACHE = {}


def kernel(**inputs):
    inp = {k: np.asarray(v) for k, v in inputs.items()}
    nt = SEQ // TT
    if nt not in _CACHE:
        _CACHE[nt] = build(nt)
    nc = _CACHE[nt]
    in_maps = [prep_inputs(inp, b, nt) for b in range(2)]
    res = run_bass_kernel_spmd(nc, in_maps, core_ids=[0, 1])
    out = np.stack([res.results[b]["outT"].T for b in range(2)], 0)
    return np.ascontiguousarray(out).astype(np.float32)
```
